# Optimizing a Trainium2 kernel written in Bass

```python
import math
import jax, jax.numpy as jnp
from jax import lax
import numpy as np

D_MODEL = 2048
BATCH = 1
SEQ = 8192
DEPTH = 2

CHUNK = 64
D_MIX = D_MODEL
ATT_HEADS = 8
ATT_HEAD_DIM = 128
D_ATT = ATT_HEADS * ATT_HEAD_DIM
Q_BLOCK = 128
SSD_HEADS = 16
SSD_HEAD_DIM = 64
D_SSD = SSD_HEADS * SSD_HEAD_DIM
SSD_GROUPS = 4
SSD_STATE = 128
CONV_WIDTH = 4
D_CONV = D_SSD + 2 * SSD_GROUPS * SSD_STATE
IN_SPLITS = (D_ATT, 2 * D_ATT, 3 * D_ATT, 3 * D_ATT + ATT_HEADS,
             3 * D_ATT + ATT_HEADS + D_SSD,
             3 * D_ATT + ATT_HEADS + D_SSD + D_CONV)
D_IN_PROJ = 3 * D_ATT + ATT_HEADS + D_SSD + D_CONV + SSD_HEADS
N_EXPERTS = 16
N_EXPERT_GROUPS = 4
EXPERTS_PER_GROUP = N_EXPERTS // N_EXPERT_GROUPS
TOP_GROUPS = 1
TOP_K = 2
D_EXPERT = 1024
PLE_DIM = 256
DEEPNORM_ALPHA = (2 * DEPTH) ** 0.25
DEEPNORM_BETA = (8 * DEPTH) ** -0.25
LN_EPS = 1e-5
RMS_EPS = 1e-5

kernel_name = "fox_ssd_hybrid_deepnorm_moe"


def layer_norm(x, g, b):
    xf = x.astype(jnp.float32)
    mu = xf.mean(-1, keepdims=True)
    var = jnp.square(xf - mu).mean(-1, keepdims=True)
    return ((xf - mu) * lax.rsqrt(var + LN_EPS) * g + b).astype(x.dtype)


def forgetting_attention(q, k, v, log_f):
    b, s, h, dh = q.shape
    f_cum = jnp.transpose(jnp.cumsum(log_f, axis=1), (0, 2, 1))
    pos = jnp.arange(s)
    scale = dh ** -0.5

    def block(i):
        start = i * Q_BLOCK
        qb = lax.dynamic_slice_in_dim(q, start, Q_BLOCK, axis=1)
        fq = lax.dynamic_slice_in_dim(f_cum, start, Q_BLOCK, axis=2)
        qpos = start + jnp.arange(Q_BLOCK)
        logits = jnp.einsum('bqhd,bkhd->bhqk', qb, k,
                            preferred_element_type=jnp.float32) * scale
        logits = logits + fq[..., :, None] - f_cum[..., None, :]
        logits = jnp.where(pos[None, :] <= qpos[:, None], logits, -jnp.inf)
        probs = jax.nn.softmax(logits, axis=-1)
        return jnp.einsum('bhqk,bkhd->bqhd', probs.astype(v.dtype), v)

    out = lax.map(block, jnp.arange(s // Q_BLOCK))
    return jnp.moveaxis(out, 0, 1).reshape(b, s, h * dh)


def causal_depthwise_conv(u, w, bias):
    c = u.shape[-1]
    k = w.shape[0]
    out = lax.conv_general_dilated(u, w[:, None, :], window_strides=(1,),
                                   padding=[(k - 1, 0)],
                                   dimension_numbers=('NWC', 'WIO', 'NWC'),
                                   feature_group_count=c)
    return out + bias


def ssd_scan(xh, dt, a, bm, cm):
    f32 = jnp.float32
    b, s, h, p = xh.shape
    g, n = bm.shape[2], bm.shape[3]
    r = h // g
    nc = s // CHUNK
    xc = xh.reshape(b, nc, CHUNK, g, r, p).astype(f32)
    dtc = dt.reshape(b, nc, CHUNK, g, r)
    bc = bm.reshape(b, nc, CHUNK, g, n).astype(f32)
    cc = cm.reshape(b, nc, CHUNK, g, n).astype(f32)
    a_cum = jnp.cumsum(dtc * a.reshape(g, r), axis=2)
    seg = a_cum[:, :, :, None] - a_cum[:, :, None, :]
    causal = jnp.tril(jnp.ones((CHUNK, CHUNK), dtype=bool))[:, :, None, None]
    decay = jnp.exp(jnp.where(causal, seg, -jnp.inf))
    cb = jnp.einsum('bcign,bcjgn->bcijg', cc, bc)
    y_diag = jnp.einsum('bcijg,bcijgr,bcjgr,bcjgrp->bcigrp', cb, decay, dtc, xc)
    decay_to_end = jnp.exp(a_cum[:, :, -1:] - a_cum)
    states = jnp.einsum('bcjgn,bcjgr,bcjgrp->bcgrpn', bc, decay_to_end * dtc, xc)
    chunk_decay = jnp.exp(a_cum[:, :, -1])

    def step(hstate, inp):
        st, dec = inp
        return hstate * dec[..., None, None] + st, hstate

    h0 = jnp.zeros((b, g, r, p, n), f32)
    _, prev = lax.scan(step, h0, (jnp.moveaxis(states, 1, 0),
                                  jnp.moveaxis(chunk_decay, 1, 0)))
    prev = jnp.moveaxis(prev, 0, 1)
    y_off = jnp.einsum('bcign,bcgrpn,bcigr->bcigrp', cc, prev, jnp.exp(a_cum))
    return (y_diag + y_off).reshape(b, s, h, p)


def hybrid_mixer(x, w_in, b_forget, conv_w, conv_b, dt_bias, a_log, d_skip,
                 ssd_norm_w, w_out):
    f32 = jnp.float32
    b, s, _ = x.shape
    proj = jnp.einsum('bsd,de->bse', x, w_in)
    q, k, v, f_logit, z, xbc, dt_raw = jnp.split(proj, IN_SPLITS, axis=-1)
    qh = q.reshape(b, s, ATT_HEADS, ATT_HEAD_DIM)
    kh = k.reshape(b, s, ATT_HEADS, ATT_HEAD_DIM)
    vh = v.reshape(b, s, ATT_HEADS, ATT_HEAD_DIM)
    log_f = jax.nn.log_sigmoid((f_logit + b_forget).astype(f32))
    att = forgetting_attention(qh, kh, vh, log_f)
    xbc = jax.nn.silu(causal_depthwise_conv(xbc, conv_w, conv_b))
    xs, bm, cm = jnp.split(xbc, (D_SSD, D_SSD + SSD_GROUPS * SSD_STATE), axis=-1)
    dt = jax.nn.softplus((dt_raw + dt_bias).astype(f32))
    a = -jnp.exp(a_log.astype(f32))
    xh = xs.reshape(b, s, SSD_HEADS, SSD_HEAD_DIM)
    y = ssd_scan(xh, dt, a,
                 bm.reshape(b, s, SSD_GROUPS, SSD_STATE),
                 cm.reshape(b, s, SSD_GROUPS, SSD_STATE))
    y = y + d_skip.astype(f32)[:, None] * xh.astype(f32)
    yg = (y.reshape(b, s, D_SSD) * jax.nn.silu(z.astype(f32))).reshape(b, s, SSD_GROUPS, -1)
    yg = yg * lax.rsqrt(jnp.mean(jnp.square(yg), -1, keepdims=True) + RMS_EPS)
    ssd = (yg.reshape(b, s, D_SSD) * ssd_norm_w).astype(x.dtype)
    mixed = jnp.concatenate([att.astype(x.dtype), ssd], axis=-1)
    return jnp.einsum('bse,ed->bsd', mixed, w_out)


def routed_experts(x, w_router, router_bias, w_gate, w_up, w_down):
    f32 = jnp.float32
    b, s, d = x.shape
    xt = x.reshape(b * s, d)
    scores = jax.nn.sigmoid(jnp.einsum('td,de->te', xt, w_router,
                                       preferred_element_type=f32))
    biased = (scores + router_bias.astype(f32)).reshape(-1, N_EXPERT_GROUPS, EXPERTS_PER_GROUP)
    group_score = lax.top_k(biased, 2)[0].sum(-1)
    _, top_group = lax.top_k(group_score, TOP_GROUPS)
    group_mask = (top_group[:, :, None] == jnp.arange(N_EXPERT_GROUPS)).any(axis=1)
    masked = jnp.where(group_mask[:, :, None], biased, -jnp.inf).reshape(-1, N_EXPERTS)
    _, top_idx = lax.top_k(masked, TOP_K)
    top_w = jnp.take_along_axis(scores, top_idx, axis=-1)
    top_w = top_w / top_w.sum(-1, keepdims=True)
    combine = jnp.einsum('tk,tke->te', top_w, jax.nn.one_hot(top_idx, N_EXPERTS, dtype=f32))
    out = jnp.zeros((b * s, d), f32)
    for e in range(N_EXPERTS):
        hid = jax.nn.silu(xt @ w_gate[e]) * (xt @ w_up[e])
        out = out + combine[:, e:e + 1] * (hid @ w_down[e])
    return out.reshape(b, s, d).astype(x.dtype)


def per_layer_embedding(x, p_i, w_ple, w_ple_gate):
    gate = jax.nn.sigmoid(jnp.einsum('bsd,de->bse', x, w_ple_gate))
    return gate * jnp.einsum('bsk,kd->bsd', p_i, w_ple)


def setup_inputs(seed: int = 0) -> dict:
    key = jax.random.key(seed)
    ks = jax.random.split(key, 24)
    f32 = jnp.float32
    nrm = lambda k, shape: jax.random.normal(k, shape, f32)
    x = nrm(ks[0], (BATCH, SEQ, D_MODEL))
    p = nrm(ks[1], (DEPTH, BATCH, SEQ, PLE_DIM))
    col_scale = jnp.concatenate([
        jnp.ones((2 * D_ATT,), f32),
        jnp.full((D_ATT,), DEEPNORM_BETA, f32),
        jnp.ones((D_IN_PROJ - 3 * D_ATT,), f32)])
    w_in = nrm(ks[2], (DEPTH, D_MODEL, D_IN_PROJ)) * D_MODEL ** -0.5 * col_scale
    b_forget = jax.random.uniform(ks[3], (DEPTH, ATT_HEADS), f32, 1.0, 5.0)
    conv_w = nrm(ks[4], (DEPTH, CONV_WIDTH, D_CONV)) * CONV_WIDTH ** -0.5
    conv_b = 0.02 * nrm(ks[5], (DEPTH, D_CONV))
    dt0 = jnp.exp(jax.random.uniform(ks[6], (DEPTH, SSD_HEADS), f32,
                                     math.log(1e-3), math.log(1e-1)))
    dt_bias = dt0 + jnp.log(-jnp.expm1(-dt0))
    a_log = jnp.log(jax.random.uniform(ks[7], (DEPTH, SSD_HEADS), f32, 1.0, 16.0))
    d_skip = 1.0 + 0.1 * nrm(ks[8], (DEPTH, SSD_HEADS))
    ssd_norm_w = 1.0 + 0.1 * nrm(ks[9], (DEPTH, D_SSD))
    w_out = nrm(ks[10], (DEPTH, D_MIX, D_MODEL)) * D_MIX ** -0.5 * DEEPNORM_BETA
    ln1_g = 1.0 + 0.05 * nrm(ks[11], (DEPTH, D_MODEL))
    ln1_b = 0.02 * nrm(ks[12], (DEPTH, D_MODEL))
    w_router = nrm(ks[13], (D_MODEL, N_EXPERTS)) * D_MODEL ** -0.5
    router_bias = 0.01 * nrm(ks[14], (N_EXPERTS,))
    w_gate = nrm(ks[15], (DEPTH, N_EXPERTS, D_MODEL, D_EXPERT)) * D_MODEL ** -0.5
    w_up = nrm(ks[16], (DEPTH, N_EXPERTS, D_MODEL, D_EXPERT)) * D_MODEL ** -0.5 * DEEPNORM_BETA
    w_down = nrm(ks[17], (DEPTH, N_EXPERTS, D_EXPERT, D_MODEL)) * D_EXPERT ** -0.5 * DEEPNORM_BETA
    w_ple = nrm(ks[18], (DEPTH, PLE_DIM, D_MODEL)) * PLE_DIM ** -0.5
    w_ple_gate = nrm(ks[19], (DEPTH, D_MODEL, D_MODEL)) * D_MODEL ** -0.5
    ln2_g = 1.0 + 0.05 * nrm(ks[20], (DEPTH, D_MODEL))
    ln2_b = 0.02 * nrm(ks[21], (DEPTH, D_MODEL))
    return {"x": x, "p": p, "w_in": w_in, "b_forget": b_forget,
            "conv_w": conv_w, "conv_b": conv_b, "dt_bias": dt_bias,
            "a_log": a_log, "d_skip": d_skip, "ssd_norm_w": ssd_norm_w,
            "w_out": w_out, "ln1_g": ln1_g, "ln1_b": ln1_b,
            "w_router": w_router, "router_bias": router_bias,
            "w_gate": w_gate, "w_up": w_up, "w_down": w_down,
            "w_ple": w_ple, "w_ple_gate": w_ple_gate,
            "ln2_g": ln2_g, "ln2_b": ln2_b}


def reference(x, p, w_in, b_forget, conv_w, conv_b, dt_bias, a_log, d_skip,
              ssd_norm_w, w_out, ln1_g, ln1_b, w_router, router_bias,
              w_gate, w_up, w_down, w_ple, w_ple_gate, ln2_g, ln2_b):
    h = x
    for i in range(DEPTH):
        mix = hybrid_mixer(h, w_in[i], b_forget[i], conv_w[i], conv_b[i],
                           dt_bias[i], a_log[i], d_skip[i], ssd_norm_w[i], w_out[i])
        h = layer_norm(DEEPNORM_ALPHA * h + mix, ln1_g[i], ln1_b[i])
        moe = routed_experts(h, w_router, router_bias, w_gate[i], w_up[i], w_down[i])
        ple = per_layer_embedding(h, p[i], w_ple[i], w_ple_gate[i])
        h = layer_norm(DEEPNORM_ALPHA * h + moe + ple, ln2_g[i], ln2_b[i])
    return h
```

```python
import numpy as np
import concourse.bass as bass
import concourse.mybir as mybir
from concourse.bass_utils import run_bass_kernel_spmd

F32 = mybir.dt.float32
BF16 = mybir.dt.bfloat16
AF = mybir.ActivationFunctionType
ALU = mybir.AluOpType
AX = mybir.AxisListType

NCORES = 8
D = 2048
SEQ = 8192
DEPTH = 2
TOK = SEQ // NCORES
NT = TOK // 128
NE = 16
DE = 1024
ALPHA = float((2 * DEPTH) ** 0.25)
LN_EPS = 1e-5
RMS_EPS = 1e-5
SB_BASE = 16512
SB_TOP = 229344
BIG = 1.0e4

ENGS = ("pe", "act", "dve", "pool", "sp")
SAME_ENG_SYNC = {"pe": False, "act": True, "dve": True, "pool": True, "sp": False}


class Unit:
    __slots__ = ("name", "space", "lo", "hi", "writer", "readers", "ovl")

    def __init__(self, name, space, lo, hi):
        self.name, self.space, self.lo, self.hi = name, space, lo, hi
        self.writer = None
        self.readers = []
        self.ovl = None


class Lane:
    def __init__(self, name):
        self.name = name
        self.sem = None
        self.count = 0


class DmaGroup:
    __slots__ = ("lane", "value")

    def __init__(self, lane):
        self.lane = lane
        self.value = 0


class Op:
    __slots__ = ("fn", "waits", "signals", "sigval", "group")

    def __init__(self, fn):
        self.fn = fn
        self.waits = []
        self.signals = False
        self.sigval = 0
        self.group = None


class Sched:
    def __init__(self):
        self.ops = {e: [] for e in ENGS}
        self.units = {"sb": [], "ps": [], "dr": []}
        self.lanes = []

    def unit(self, name, space, lo, hi):
        u = Unit(name, space, lo, hi)
        self.units[space].append(u)
        for v in self.units[space]:
            v.ovl = None
        return u

    def lane(self, name):
        l = Lane(name)
        self.lanes.append(l)
        return l

    def _ovl(self, u):
        if u.ovl is None:
            u.ovl = [v for v in self.units[u.space] if v.lo < u.hi and u.lo < v.hi]
        return u.ovl

    def add(self, eng, fn, reads=(), writes=(), group=None):
        op = Op(fn)
        idx = len(self.ops[eng])
        deps = []
        for u in reads:
            for v in self._ovl(u):
                if v.writer is not None:
                    deps.append(v.writer)
        for u in writes:
            for v in self._ovl(u):
                if v.writer is not None:
                    deps.append(v.writer)
                deps.extend(v.readers)
        if group is not None:
            myev = group
            group.lane.count += 16
            group.value = group.lane.count
            op.group = group
        else:
            myev = (eng, idx)
        seen = set()
        for d in deps:
            if d is myev:
                continue
            if isinstance(d, tuple):
                if d[0] == eng and not SAME_ENG_SYNC[eng]:
                    continue
                if d in seen:
                    continue
                seen.add(d)
                self.ops[d[0]][d[1]].signals = True
                op.waits.append(d)
            else:
                if id(d) in seen:
                    continue
                seen.add(id(d))
                op.waits.append(d)
        self.ops[eng].append(op)
        for u in writes:
            u.writer = myev
            u.readers = []
        for u in reads:
            if isinstance(myev, tuple):
                u.readers = [r for r in u.readers
                             if not (isinstance(r, tuple) and r[0] == eng)]
            u.readers.append(myev)
        return myev

    def emit(self, block, sems):
        for e in ENGS:
            c = 0
            for op in self.ops[e]:
                if op.signals:
                    c += 1
                    op.sigval = c
        final_lanes = [(l.sem, l.count) for l in self.lanes if l.count > 0]
        ops = self.ops

        def run(e, engobj):
            waited = {}
            for op in ops[e]:
                for d in op.waits:
                    if isinstance(d, tuple):
                        sem, val = sems[d[0]], ops[d[0]][d[1]].sigval
                    else:
                        sem, val = d.lane.sem, d.value
                    k = id(sem)
                    if waited.get(k, 0) >= val:
                        continue
                    waited[k] = val
                    engobj.wait_ge(sem, val)
                inst = op.fn(engobj)
                if op.group is not None:
                    inst.then_inc(op.group.lane.sem, 16)
                elif op.signals:
                    inst.then_inc(sems[e], 1)
            if e == "sp":
                for sem, cnt in final_lanes:
                    engobj.wait_ge(sem, cnt)

        @block.sync
        def _(eng):
            run("sp", eng)

        @block.tensor
        def _(eng):
            run("pe", eng)

        @block.scalar
        def _(eng):
            run("act", eng)

        @block.vector
        def _(eng):
            run("dve", eng)

        @block.gpsimd
        def _(eng):
            run("pool", eng)


class Ctx:
    def __init__(self):
        self.nc = bass.Bass("TRN2", target_bir_lowering=False)
        self.S = Sched()
        self.n_names = 0
        self.banks = []
        for b in range(8):
            t = self.nc.alloc_psum_tensor("psb%d" % b, [128, 512], F32)
            self.banks.append((t, self.S.unit("psb%d" % b, "ps", b * 2048, (b + 1) * 2048)))
        self.rot = list(range(8))
        self.bank_i = 0

    def dram_in(self, name, shape, dtype=F32):
        return self.nc.dram_tensor(name, list(shape), dtype, kind="ExternalInput").ap()

    def dram_out(self, name, shape, dtype=F32):
        return self.nc.dram_tensor(name, list(shape), dtype, kind="ExternalOutput").ap()

    def sb(self, name, shape, dtype, off):
        esz = 2 if dtype == BF16 else 4
        n = esz
        for s in shape[1:]:
            n *= s
        assert off >= SB_BASE and off + n <= SB_TOP, (name, off, n)
        self.n_names += 1
        t = self.nc.alloc_sbuf_tensor_at("%s_%d" % (name, self.n_names), list(shape), dtype, offset=off)
        return t, n

    def sbu(self, name, shape, dtype, off):
        t, n = self.sb(name, shape, dtype, off)
        return t, self.S.unit(name, "sb", off, off + n)

    def next_bank(self):
        b = self.banks[self.rot[self.bank_i % len(self.rot)]]
        self.bank_i += 1
        return b

    def reserve_bank(self):
        i = self.rot.pop()
        return i, self.banks[i]

    def release_bank(self, i):
        self.rot.append(i)

    def finish(self):
        nc = self.nc
        S = self.S
        sem_cms = [nc.semaphore("sem_" + e) for e in ENGS] + [nc.semaphore("lane_%d" % i) for i in range(len(S.lanes))]
        handles = [cm.__enter__() for cm in sem_cms]
        sems = dict(zip(ENGS, handles[:len(ENGS)]))
        for l, h in zip(S.lanes, handles[len(ENGS):]):
            l.sem = h
        with nc.Block() as block:
            S.emit(block, sems)
        return nc


def layer_norm_tile(C, x_ap, x_units, out_ap, out_units, g_t, b_t, gb_units, scr, scr_units, tag):
    S = C.S
    stats = scr[:, 0:24].rearrange("p (c s) -> p c s", c=4)
    for c in range(4):
        S.add("dve", lambda e, c=c: e.bn_stats(out=stats[:, c, :], in_=x_ap[:, c * 512:(c + 1) * 512]),
              reads=x_units, writes=scr_units)
    mv = scr[:, 24:26]
    S.add("dve", lambda e: e.bn_aggr(out=mv, in_=scr[:, 0:24]), reads=scr_units, writes=scr_units)
    rstd = scr[:, 26:27]
    S.add("act", lambda e: e.activation(out=rstd, in_=scr[:, 25:26], func=AF.Sqrt, bias=LN_EPS_AP[0], scale=1.0),
          reads=scr_units, writes=scr_units)
    S.add("dve", lambda e: e.reciprocal(out=rstd, in_=rstd), reads=scr_units, writes=scr_units)
    S.add("dve", lambda e: e.tensor_scalar(out=out_ap, in0=x_ap, scalar1=scr[:, 24:25], scalar2=rstd,
                                           op0=ALU.subtract, op1=ALU.mult),
          reads=x_units + scr_units, writes=out_units)
    S.add("pool", lambda e: e.tensor_tensor(out=out_ap, in0=out_ap, in1=g_t[:], op=ALU.mult),
          reads=out_units + gb_units, writes=out_units)
    S.add("pool", lambda e: e.tensor_tensor(out=out_ap, in0=out_ap, in1=b_t[:], op=ALU.add),
          reads=out_units + gb_units, writes=out_units)


LN_EPS_AP = [None]


def build_stage_b(C, T, stop=None, ne_run=NE):
    nc, S = C.nc, C.S
    o = SB_BASE
    X_OFF = o; o += 65536
    Y_OFF = o; o += 65536
    H1T_OFF = o; o += 32768
    HID_OFF = o; o += 16384
    M_OFF = o
    Xt, _ = C.sb("X", [128, NT, D], F32, X_OFF)
    xu = [[S.unit("X%d_%d" % (t, q), "sb", X_OFF + t * 8192 + q * 2048, X_OFF + t * 8192 + (q + 1) * 2048)
           for q in range(4)] for t in range(NT)]
    Yt, _ = C.sb("Y", [128, NT, D], F32, Y_OFF)
    yu = [[S.unit("Y%d_%d" % (t, q), "sb", Y_OFF + t * 8192 + q * 2048, Y_OFF + t * 8192 + (q + 1) * 2048)
           for q in range(4)] for t in range(NT)]
    mixT, u_mixT = C.sbu("mixT", [128, 16, TOK], BF16, Y_OFF)
    ygf, u_ygf = C.sbu("ygf", [128, 8, TOK], F32, Y_OFF + 32768)
    h1T, u_h1T = C.sbu("h1T", [128, 16, TOK], BF16, H1T_OFF)
    wo = [C.sbu("wo%d" % i, [128, 16, 512], BF16, H1T_OFF + i * 16384) for i in range(2)]
    hidT, u_hidT = C.sbu("hidT", [128, 8, TOK], BF16, HID_OFF)
    lng, u_lng = C.sbu("lng", [128, D], F32, HID_OFF)
    lnb, u_lnb = C.sbu("lnb", [128, D], F32, HID_OFF + 8192)
    m = M_OFF
    ident, u_ident = C.sbu("ident", [128, 128], F32, m); m += 512
    wr, u_wr = C.sbu("wr", [128, 16, 16], F32, m); m += 1024
    rb, u_rb = C.sbu("rb", [128, 16], F32, m); m += 64
    eps_t, u_eps = C.sbu("eps", [128, 1], F32, m); m += 32
    snw, u_snw = C.sbu("snw", [128, 8], F32, m); m += 32
    ones_f, u_onesf = C.sbu("ones_fB", [128, 128], F32, m); m += 512
    lnscr = [C.sbu("lnscr%d" % i, [128, 32], F32, m + 128 * i) for i in range(2)]; m += 256
    sc, u_sc = C.sbu("sc", [128, NT * 16], F32, m); m += 512
    bi, u_bi = C.sbu("bi", [128, NT * 16], F32, m); m += 512
    tm, u_tm = C.sbu("tm", [128, NT * 16], F32, m); m += 512
    comb, u_comb = C.sbu("comb", [128, NT * 16], F32, m); m += 512
    g4, u_g4 = C.sbu("g4", [128, 6, NT * 4], F32, m); m += 768
    g1, u_g1 = C.sbu("g1", [128, 2, NT], F32, m); m += 64
    h1f = [C.sbu("h1f%d" % i, [128, 4, 128], F32, m + 2048 * i) for i in range(2)]; m += 4096
    sig = [C.sbu("sig%d" % i, [128, 512], F32, m + 2048 * i) for i in range(2)]; m += 4096
    tmp = [C.sbu("tmp%d" % i, [128, 512], F32, m + 2048 * i) for i in range(2)]; m += 4096
    pT, u_pT = C.sbu("pT", [128, 2, TOK], BF16, m); m += 4096
    assert m <= SB_TOP, m
    NGU = 4
    gu = [C.sbu("gu%d" % i, [128, 2, 16, 128], BF16, X_OFF + i * 8192) for i in range(NGU)]
    NDN = 3
    dn = [C.sbu("dn%d" % i, [128, 8, 512], BF16, X_OFF + 32768 + i * 8192) for i in range(NDN)]
    wple, u_wple = C.sbu("wple", [128, 2, D], BF16, X_OFF + 57344)
    wpg = [C.sbu("wpg%d" % i, [128, 16, 512], BF16, X_OFF + i * 16384) for i in range(2)]
    gu_l = [S.lane("gu%d" % i) for i in range(NGU)]
    dn_l = [S.lane("dn%d" % i) for i in range(NDN)]
    wo_l = [S.lane("wo%d" % i) for i in range(2)]
    wpg_l = [S.lane("wpg%d" % i) for i in range(2)]
    c_l = S.lane("const")
    x_l = [S.lane("x%d" % t) for t in range(NT)]
    o_l = [S.lane("o%d" % t) for t in range(NT)]
    mix_l = S.lane("mix")

    cg = DmaGroup(c_l)
    S.add("sp", lambda e: e.dma_start(out=ident[:], in_=T["ident"]), writes=[u_ident], group=cg)
    S.add("sp", lambda e: e.dma_start(out=wr[:], in_=T["wr"]), writes=[u_wr], group=cg)
    S.add("sp", lambda e: e.dma_start(out=rb[:], in_=T["rbias"].broadcast_to([128, 16])), writes=[u_rb], group=cg)
    S.add("sp", lambda e: e.dma_start(out=lng[:], in_=T["ln1g"].broadcast_to([128, D])), writes=[u_lng], group=cg)
    S.add("sp", lambda e: e.dma_start(out=lnb[:], in_=T["ln1b"].broadcast_to([128, D])), writes=[u_lnb], group=cg)
    S.add("dve", lambda e: e.memset(eps_t[:], LN_EPS), writes=[u_eps])
    LN_EPS_AP[0] = eps_t[:]
    for t in range(NT):
        S.add("sp", lambda e, t=t: e.dma_start(out=Xt[:, t, :], in_=T["h"][t * 128:(t + 1) * 128, :]),
              writes=xu[t], group=DmaGroup(x_l[t]))
    mg = DmaGroup(mix_l)
    for c4 in range(2):
        S.add("pool", lambda e, c4=c4: e.dma_start(
            out=mixT[:, c4 * 4:(c4 + 1) * 4, :],
            in_=T["mixT"][c4 * 512:(c4 + 1) * 512, :].rearrange("(c p) t -> p c t", p=128)),
            writes=[u_mixT], group=mg)
    yg_g = DmaGroup(S.lane("ygf"))
    for c4 in range(2):
        S.add("sp", lambda e, c4=c4: e.dma_start(
            out=ygf[:, c4 * 4:(c4 + 1) * 4, :],
            in_=T["mixT"][1024 + c4 * 512:1024 + (c4 + 1) * 512, :].rearrange("(c p) t -> p c t", p=128)),
            writes=[u_ygf], group=yg_g)
    S.add("sp", lambda e: e.dma_start(out=snw[:], in_=T["snw"]), writes=[u_snw], group=cg)
    S.add("dve", lambda e: e.memset(ones_f[:], 1.0), writes=[u_onesf])
    for g in range(4):
        for th in range(2):
            ts_ = slice(th * 512, (th + 1) * 512)
            mbk, mbu = C.next_bank()
            for k in range(2):
                sq, squ = sig[k]
                S.add("act", lambda e, g=g, k=k, sq=sq, ts_=ts_: e.activation(out=sq[:], in_=ygf[:, 2 * g + k, ts_], func=AF.Square),
                      reads=[u_ygf], writes=[squ])
                S.add("pe", lambda e, k=k, sq=sq, mbk=mbk: e.matmul(mbk[:], lhsT=ones_f[:], rhs=sq[:], start=(k == 0), stop=(k == 1)),
                      reads=[u_onesf, squ], writes=[mbu])
            rs, rsu = tmp[0]
            S.add("act", lambda e, mbk=mbk, rs=rs: e.activation(out=rs[:], in_=mbk[:], func=AF.Sqrt, bias=eps_t[:], scale=1.0 / 256.0),
                  reads=[mbu, u_eps], writes=[rsu])
            S.add("dve", lambda e, rs=rs: e.reciprocal(out=rs[:], in_=rs[:]), reads=[rsu], writes=[rsu])
            for k in range(2):
                S.add("dve", lambda e, g=g, k=k, rs=rs, ts_=ts_: e.scalar_tensor_tensor(
                    out=mixT[:, 8 + 2 * g + k, ts_], in0=ygf[:, 2 * g + k, ts_], scalar=snw[:, 2 * g + k:2 * g + k + 1], in1=rs[:],
                    op0=ALU.mult, op1=ALU.mult), reads=[u_ygf, u_snw, rsu], writes=[u_mixT])
    S.add("pool", lambda e: e.dma_start(out=pT[:], in_=T["pT"].rearrange("(c p) t -> p c t", p=128)),
          writes=[u_pT], group=cg)

    for dq in range(4):
        wt, wu_ = wo[dq % 2]
        S.add("pool", lambda e, dq=dq, wt=wt: e.dma_start(out=wt[:], in_=T["wout"][dq]),
              writes=[wu_], group=DmaGroup(wo_l[dq % 2]))
        for t in range(NT):
            bk, bu = C.next_bank()
            for kc in range(16):
                S.add("pe", lambda e, kc=kc, t=t, bk=bk, wt=wt: e.matmul(
                    bk[:], lhsT=mixT[:, kc, t * 128:(t + 1) * 128], rhs=wt[:, kc, :],
                    start=(kc == 0), stop=(kc == 15)), reads=[u_mixT, wu_], writes=[bu])
            xs = Xt[:, t, dq * 512:(dq + 1) * 512]
            S.add("dve", lambda e, xs=xs, bk=bk: e.scalar_tensor_tensor(
                out=xs, in0=xs, scalar=ALPHA, in1=bk[:], op0=ALU.mult, op1=ALU.add),
                reads=[bu, xu[t][dq]], writes=[xu[t][dq]])

    def dump(src):
        for t in range(NT):
            S.add("sp", lambda e, t=t: e.dma_start(out=T["hout"][t * 128:(t + 1) * 128, :], in_=src[:, t, :]),
                  reads=xu[t] + yu[t], group=DmaGroup(o_l[t]))
    if stop == "p2a":
        return dump(Xt)
    rbi, (rbk, rbu) = C.reserve_bank()
    for t in range(NT):
        scr, scr_u = lnscr[t % 2]
        xt = Xt[:, t, :]
        layer_norm_tile(C, xt, xu[t], xt, xu[t], lng, lnb, [u_lng, u_lnb], scr, [scr_u], "ln1_%d" % t)
        if stop == "ln1":
            continue
        S.add("act", lambda e, t=t, xt=xt: e.activation(out=Yt[:, t, :], in_=xt, func=AF.Copy, scale=ALPHA),
              reads=xu[t], writes=yu[t])
        if stop == "acc":
            continue
        for kg in range(4):
            bk, bu = C.next_bank()
            for j in range(4):
                kc = kg * 4 + j
                S.add("pe", lambda e, j=j, kc=kc, bk=bk, xt=xt: e.transpose(
                    out=bk[:, j * 128:(j + 1) * 128], in_=xt[:, kc * 128:(kc + 1) * 128], identity=ident[:]),
                    reads=xu[t] + [u_ident], writes=[bu])
            hf, hfu = h1f[kg % 2]
            if stop == "tr0":
                S.add("act", lambda e, bk=bk, kg=kg, t=t: e.copy(out=Yt[:, t, kg * 512:(kg + 1) * 512], in_=bk[:]), reads=[bu], writes=yu[t])
                continue
            S.add("act", lambda e, bk=bk, kg=kg, t=t: e.copy(
                out=h1T[:, kg * 4:(kg + 1) * 4, t * 128:(t + 1) * 128],
                in_=bk[:].rearrange("p (j c) -> p j c", j=4)), reads=[bu], writes=[u_h1T])
            if stop == "tr1":
                continue
            S.add("act", lambda e, bk=bk, hf=hf: e.copy(
                out=hf[:].rearrange("p j c -> p (j c)"), in_=bk[:]), reads=[bu], writes=[hfu])
            if stop == "tr2":
                continue
            for j in range(4):
                kc = kg * 4 + j
                S.add("pe", lambda e, j=j, kc=kc, hf=hf, t=t: e.matmul(
                    rbk[:, t * 16:(t + 1) * 16], lhsT=hf[:, j, :], rhs=wr[:, kc, :],
                    start=(kc == 0), stop=(kc == 15)), reads=[hfu, u_wr], writes=[rbu])

    if stop in ("p2b", "ln1", "acc", "tr0", "tr1", "tr2"):
        return dump(Xt)
    NG = NT * 4
    def v3(ap):
        return ap.rearrange("p (g j) -> p g j", j=4)
    S.add("act", lambda e: e.activation(out=sc[:], in_=rbk[:, 0:NT * 16], func=AF.Sigmoid), reads=[rbu], writes=[u_sc])
    S.add("dve", lambda e: e.tensor_tensor(
        out=bi[:].rearrange("p (t x) -> p t x", x=16), in0=sc[:].rearrange("p (t x) -> p t x", x=16),
        in1=rb[:].rearrange("p (o x) -> p o x", o=1).broadcast_to([128, NT, 16]), op=ALU.add),
        reads=[u_sc, u_rb], writes=[u_bi])
    m1, m2, gs, gm, gt = (g4[:, i, :] for i in range(5))
    def bc(ap):
        return ap.rearrange("p (g o) -> p g o", o=1).broadcast_to([128, NG, 4])
    S.add("dve", lambda e: e.tensor_reduce(out=m1, in_=v3(bi[:]), axis=AX.X, op=ALU.max), reads=[u_bi], writes=[u_g4])
    S.add("dve", lambda e: e.tensor_tensor(out=v3(tm[:]), in0=v3(bi[:]), in1=bc(m1), op=ALU.is_ge), reads=[u_bi, u_g4], writes=[u_tm])
    S.add("dve", lambda e: e.scalar_tensor_tensor(out=tm[:], in0=tm[:], scalar=-BIG, in1=bi[:], op0=ALU.mult, op1=ALU.add),
          reads=[u_tm, u_bi], writes=[u_tm])
    S.add("dve", lambda e: e.tensor_reduce(out=m2, in_=v3(tm[:]), axis=AX.X, op=ALU.max), reads=[u_tm], writes=[u_g4])
    S.add("dve", lambda e: e.tensor_tensor(out=gs, in0=m1, in1=m2, op=ALU.add), reads=[u_g4], writes=[u_g4])
    gmax = g1[:, 0, :]
    S.add("dve", lambda e: e.tensor_reduce(out=gmax, in_=gs.rearrange("p (t g) -> p t g", g=4), axis=AX.X, op=ALU.max),
          reads=[u_g4], writes=[u_g1])
    S.add("dve", lambda e: e.tensor_tensor(
        out=gm.rearrange("p (t g) -> p t g", g=4), in0=gs.rearrange("p (t g) -> p t g", g=4),
        in1=gmax.rearrange("p (t o) -> p t o", o=1).broadcast_to([128, NT, 4]), op=ALU.is_ge),
        reads=[u_g4, u_g1], writes=[u_g4])
    S.add("dve", lambda e: e.tensor_tensor(out=v3(tm[:]), in0=v3(bi[:]), in1=bc(m2), op=ALU.is_ge), reads=[u_bi, u_g4], writes=[u_tm])
    S.add("dve", lambda e: e.tensor_tensor(out=v3(tm[:]), in0=v3(tm[:]), in1=bc(gm), op=ALU.mult), reads=[u_tm, u_g4], writes=[u_tm])
    S.add("dve", lambda e: e.tensor_tensor(out=tm[:], in0=tm[:], in1=sc[:], op=ALU.mult), reads=[u_tm, u_sc], writes=[u_tm])
    den = g1[:, 1, :]
    S.add("dve", lambda e: e.tensor_reduce(out=den, in_=tm[:].rearrange("p (t x) -> p t x", x=16), axis=AX.X, op=ALU.add),
          reads=[u_tm], writes=[u_g1])
    S.add("dve", lambda e: e.reciprocal(out=den, in_=den), reads=[u_g1], writes=[u_g1])
    S.add("dve", lambda e: e.tensor_tensor(
        out=comb[:].rearrange("p (t x) -> p t x", x=16), in0=tm[:].rearrange("p (t x) -> p t x", x=16),
        in1=den.rearrange("p (t o) -> p t o", o=1).broadcast_to([128, NT, 16]), op=ALU.mult),
        reads=[u_tm, u_g1], writes=[u_comb])

    C.release_bank(rbi)
    if stop == "route":
        S.add("dve", lambda e: e.tensor_copy(out=Yt[:, 0, 0:NT * 16], in_=comb[:]), reads=[u_comb], writes=yu[0])
        return dump(Yt)
    gu_i = 0
    dn_i = 0
    for ex in range(ne_run):
        for fc in range(8):
            gt_, gu_u = gu[gu_i % NGU]
            grp = DmaGroup(gu_l[gu_i % NGU])
            gu_i += 1
            S.add("pool", lambda e, ex=ex, fc=fc, gt_=gt_: e.dma_start(out=gt_[:, 0, :, :], in_=T["wg"][ex, fc]),
                  writes=[gu_u], group=grp)
            S.add("pool", lambda e, ex=ex, fc=fc, gt_=gt_: e.dma_start(out=gt_[:, 1, :, :], in_=T["wu"][ex, fc]),
                  writes=[gu_u], group=grp)
            bks = [C.next_bank() for _ in range(4)]
            for gi in range(2):
                for kc in range(16):
                    for th in range(2):
                        bk, bu = bks[gi * 2 + th]
                        S.add("pe", lambda e, gi=gi, kc=kc, th=th, bk=bk, gt_=gt_: e.matmul(
                            bk[:], lhsT=gt_[:, gi, kc, :], rhs=h1T[:, kc, th * 512:(th + 1) * 512],
                            start=(kc == 0), stop=(kc == 15)), reads=[gu_u, u_h1T], writes=[bu])
            for th in range(2):
                gbk, gbu = bks[th]
                ubk, ubu = bks[2 + th]
                sg, sgu = sig[th]
                tp, tpu = tmp[th]
                S.add("act", lambda e, gbk=gbk, sg=sg: e.activation(out=sg[:], in_=gbk[:], func=AF.Sigmoid),
                      reads=[gbu], writes=[sgu])
                S.add("dve", lambda e, gbk=gbk, sg=sg, tp=tp: e.tensor_tensor(out=tp[:], in0=gbk[:], in1=sg[:], op=ALU.mult),
                      reads=[gbu, sgu], writes=[tpu])
                S.add("dve", lambda e, ubk=ubk, tp=tp, fc=fc, th=th: e.tensor_tensor(
                    out=hidT[:, fc, th * 512:(th + 1) * 512], in0=ubk[:], in1=tp[:], op=ALU.mult),
                    reads=[ubu, tpu], writes=[u_hidT])
        for dq in range(4):
            dt_, dn_u = dn[dn_i % NDN]
            grp = DmaGroup(dn_l[dn_i % NDN])
            dn_i += 1
            S.add("pool", lambda e, ex=ex, dq=dq, dt_=dt_: e.dma_start(out=dt_[:], in_=T["wd"][ex, dq]),
                  writes=[dn_u], group=grp)
            for t in range(NT):
                bk, bu = C.next_bank()
                for fc in range(8):
                    S.add("pe", lambda e, fc=fc, t=t, bk=bk, dt_=dt_: e.matmul(
                        bk[:], lhsT=hidT[:, fc, t * 128:(t + 1) * 128], rhs=dt_[:, fc, :],
                        start=(fc == 0), stop=(fc == 7)), reads=[u_hidT, dn_u], writes=[bu])
                ys = Yt[:, t, dq * 512:(dq + 1) * 512]
                S.add("dve", lambda e, ys=ys, bk=bk, t=t, ex=ex: e.scalar_tensor_tensor(
                    out=ys, in0=bk[:], scalar=comb[:, t * 16 + ex:t * 16 + ex + 1], in1=ys, op0=ALU.mult, op1=ALU.add),
                    reads=[bu, u_comb, yu[t][dq]], writes=[yu[t][dq]])

    if stop == "moe":
        return dump(Yt)
    S.add("pool", lambda e: e.dma_start(out=wple[:], in_=T["wple"]), writes=[u_wple], group=DmaGroup(S.lane("wple")))
    S.add("sp", lambda e: e.dma_start(out=lng[:], in_=T["ln2g"].broadcast_to([128, D])), writes=[u_lng], group=DmaGroup(S.lane("ln2g")))
    S.add("sp", lambda e: e.dma_start(out=lnb[:], in_=T["ln2b"].broadcast_to([128, D])), writes=[u_lnb], group=DmaGroup(S.lane("ln2b")))
    for dq in range(4):
        wt, wu_ = wpg[dq % 2]
        S.add("pool", lambda e, dq=dq, wt=wt: e.dma_start(out=wt[:], in_=T["wpg"][dq]),
              writes=[wu_], group=DmaGroup(wpg_l[dq % 2]))
        for t in range(NT):
            gbk, gbu = C.next_bank()
            pbk, pbu = C.next_bank()
            for kc in range(16):
                S.add("pe", lambda e, kc=kc, t=t, gbk=gbk, wt=wt: e.matmul(
                    gbk[:], lhsT=h1T[:, kc, t * 128:(t + 1) * 128], rhs=wt[:, kc, :],
                    start=(kc == 0), stop=(kc == 15)), reads=[u_h1T, wu_], writes=[gbu])
            for kc in range(2):
                S.add("pe", lambda e, kc=kc, t=t, pbk=pbk, dq=dq: e.matmul(
                    pbk[:], lhsT=pT[:, kc, t * 128:(t + 1) * 128], rhs=wple[:, kc, dq * 512:(dq + 1) * 512],
                    start=(kc == 0), stop=(kc == 1)), reads=[u_pT, u_wple], writes=[pbu])
            sg, sgu = sig[t % 2]
            tp, tpu = tmp[t % 2]
            S.add("act", lambda e, gbk=gbk, sg=sg: e.activation(out=sg[:], in_=gbk[:], func=AF.Sigmoid),
                  reads=[gbu], writes=[sgu])
            S.add("dve", lambda e, pbk=pbk, sg=sg, tp=tp: e.tensor_tensor(out=tp[:], in0=pbk[:], in1=sg[:], op=ALU.mult),
                  reads=[pbu, sgu], writes=[tpu])
            ys = Yt[:, t, dq * 512:(dq + 1) * 512]
            S.add("pool", lambda e, ys=ys, tp=tp: e.tensor_tensor(out=ys, in0=ys, in1=tp[:], op=ALU.add),
                  reads=[tpu, yu[t][dq]], writes=[yu[t][dq]])

    if stop == "ple":
        return dump(Yt)
    for t in range(NT):
        scr, scr_u = lnscr[t % 2]
        yt = Yt[:, t, :]
        layer_norm_tile(C, yt, yu[t], yt, yu[t], lng, lnb, [u_lng, u_lnb], scr, [scr_u], "ln2_%d" % t)
        S.add("sp", lambda e, t=t, yt=yt: e.dma_start(out=T["hout"][t * 128:(t + 1) * 128, :], in_=yt),
              reads=yu[t], group=DmaGroup(o_l[t]))


def stage_b_program(stop=None, ne_run=NE):
    C = Ctx()
    T = {}
    T["h"] = C.dram_in("h", [TOK, D])
    T["mixT"] = C.dram_in("mixT", [D, TOK])
    T["pT"] = C.dram_in("pT", [256, TOK])
    T["wout"] = C.dram_in("wout", [4, 128, 16, 512])
    T["ln1g"] = C.dram_in("ln1g", [1, D]); T["ln1b"] = C.dram_in("ln1b", [1, D])
    T["ln2g"] = C.dram_in("ln2g", [1, D]); T["ln2b"] = C.dram_in("ln2b", [1, D])
    T["wr"] = C.dram_in("wr", [128, 16, 16])
    T["rbias"] = C.dram_in("rbias", [1, 16])
    ne = ne_run if stop in (None, "moe", "ple") else 1
    T["wg"] = C.dram_in("wg", [ne, 8, 128, 16, 128])
    T["wu"] = C.dram_in("wu", [ne, 8, 128, 16, 128])
    T["wd"] = C.dram_in("wd", [ne, 4, 128, 8, 512])
    T["wpg"] = C.dram_in("wpg", [4, 128, 16, 512])
    T["wple"] = C.dram_in("wple", [128, 2, D])
    T["ident"] = C.dram_in("ident", [128, 128])
    T["snw"] = C.dram_in("snw", [128, 8])
    T["hout"] = C.dram_out("hout", [TOK, D])
    build_stage_b(C, T, stop, ne_run)
    return C.finish()


def stage_b_weights(inp, i):
    f = np.ascontiguousarray
    W = {}
    W["wout"] = f(inp["w_out"][i].reshape(16, 128, 4, 512).transpose(2, 1, 0, 3))
    W["ln1g"] = f(inp["ln1_g"][i].reshape(1, D)); W["ln1b"] = f(inp["ln1_b"][i].reshape(1, D))
    W["ln2g"] = f(inp["ln2_g"][i].reshape(1, D)); W["ln2b"] = f(inp["ln2_b"][i].reshape(1, D))
    W["wr"] = f(inp["w_router"].reshape(16, 128, 16).transpose(1, 0, 2))
    W["rbias"] = f(inp["router_bias"].reshape(1, 16))
    W["wg"] = f(inp["w_gate"][i].reshape(NE, 16, 128, 8, 128).transpose(0, 3, 2, 1, 4))
    W["wu"] = f(inp["w_up"][i].reshape(NE, 16, 128, 8, 128).transpose(0, 3, 2, 1, 4))
    W["wd"] = f(inp["w_down"][i].reshape(NE, 8, 128, 4, 512).transpose(0, 3, 2, 1, 4))
    W["wpg"] = f(inp["w_ple_gate"][i].reshape(16, 128, 4, 512).transpose(2, 1, 0, 3))
    W["wple"] = f(inp["w_ple"][i].reshape(2, 128, D).transpose(1, 0, 2))
    W["ident"] = np.eye(128, dtype=np.float32)
    W["snw"] = f(inp["ssd_norm_w"][i].reshape(8, 128).T)
    return W


def run_stage_b(nc, inp, i, h, mixedT, ne=NE):
    W = stage_b_weights(inp, i)
    for k in ("wg", "wu", "wd"):
        W[k] = W[k][:ne]
    p = inp["p"][i, 0]
    in_maps = []
    for c in range(NCORES):
        sl = slice(c * TOK, (c + 1) * TOK)
        m = dict(W)
        m["h"] = np.ascontiguousarray(h[sl])
        m["mixT"] = np.ascontiguousarray(mixedT[:, sl])
        m["pT"] = np.ascontiguousarray(p[sl].T)
        in_maps.append(m)
    res = run_bass_kernel_spmd(nc, in_maps, core_ids=list(range(NCORES)))
    return np.concatenate([r["hout"] for r in res.results], axis=0)


NSB = SEQ // 512
NQB = SEQ // 128
ATT_SCALE = float(128 ** -0.5)


def build_stage_a(C, T):
    nc, S = C.nc, C.S
    A = S.add
    o = SB_BASE
    def take(n):
        nonlocal o
        r = o
        o += n
        return r
    qT, u_qT = C.sbu("qT", [128, SEQ], BF16, take(16384))
    kT, u_kT = C.sbu("kT", [128, SEQ], BF16, take(16384))
    Vt, u_Vt = C.sbu("Vt", [128, NQB, 128], BF16, take(16384))
    wA, u_wA = C.sbu("wA", [128, 16, 896], BF16, take(28672))
    wA2, u_wA2 = C.sbu("wA2", [128, 16, 4], BF16, take(128))
    hp = [C.sbu("hp%d" % i, [128, 16, 512], BF16, take(16384)) for i in range(2)]
    hp_l = [S.lane("hp%d" % i) for i in range(2)]
    cbuf = [C.sbu("cb%d" % g, [128, 516], F32, take(2080)) for g in range(3)]
    ident, u_ident = C.sbu("identA", [128, 128], F32, take(512))
    maskT, u_mask = C.sbu("maskT", [128, 128], BF16, take(256))
    maskF, u_maskF = C.sbu("maskF", [128, 128], F32, take(512))
    ones_bf, u_ones = C.sbu("ones_bf", [128, 128], BF16, take(256))
    ones_f, u_onesf = C.sbu("ones_f", [128, 128], F32, take(512))
    sel = [C.sbu("sel%d" % h, [2, 128], F32, take(512)) for h in range(2)]
    convw, u_convw = C.sbu("convw", [128, 3, 4], F32, take(64))
    convb, u_convb = C.sbu("convb", [128, 3], F32, take(32))
    dskc, u_dskc = C.sbu("dskc", [128, 1], F32, take(32))
    bfc, u_bfc = C.sbu("bfc", [1, 1], F32, take(32))
    dtb, u_dtb = C.sbu("dtb", [2, 1], F32, take(32))
    nega, u_nega = C.sbu("nega", [2, 1], F32, take(32))
    one1, u_one1 = C.sbu("one1", [2, 1], F32, take(32))
    cT, u_cT = C.sbu("cT", [128, NQB], F32, take(256))
    Frow, u_Frow = C.sbu("Frow", [1, NQB + 4], F32, take(288))
    Frep, u_Frep = C.sbu("Frep", [128, NQB + 4], F32, take(288))
    clast, u_clast = C.sbu("clast", [1, 1], F32, take(32))
    frow, u_frow = C.sbu("frow", [1, 512], F32, take(2048))
    crow, u_crow = C.sbu("crow", [1, 512], F32, take(2048))
    dtr, u_dtr = C.sbu("dtr", [2, 512], F32, take(2048))
    dAr, u_dAr = C.sbu("dAr", [2, 512], F32, take(2048))
    acr, u_acr = C.sbu("acr", [2, 512], F32, take(2048))
    onesrow, u_onesrow = C.sbu("onesrow", [2, 512], F32, take(2048))
    alast, u_alast = C.sbu("alast", [2, 4], F32, take(32))
    colA, u_colA = C.sbu("colA", [128, 4, 2], F32, take(32))
    colD, u_colD = C.sbu("colD", [128, 4, 2], F32, take(32))
    colL, u_colL = C.sbu("colL", [128, 2, 4], F32, take(32))
    colW, u_colW = C.sbu("colW", [128, 4, 2], F32, take(32))
    colE, u_colE = C.sbu("colE", [128, 2, 4], F32, take(32))
    vtmp, u_vtmp = C.sbu("vtmp", [128, 512], F32, take(2048))
    zsig, u_zsig = C.sbu("zsig", [128, 512], F32, take(2048))
    zsT, u_zsT = C.sbu("zsT", [128, 512], F32, take(2048))
    cacc = [C.sbu("cacc%d" % g, [128, 512], F32, take(2048)) for g in range(3)]
    csig = [C.sbu("csig%d" % g, [128, 512], F32, take(2048)) for g in range(3)]
    cout = [C.sbu("cout%d" % g, [128, 512], F32, take(2048)) for g in range(3)]
    BTb, u_BTb = C.sbu("BTb", [128, 512], BF16, take(1024))
    CTb, u_CTb = C.sbu("CTb", [128, 512], BF16, take(1024))
    xtok, u_xtok = C.sbu("xtok", [128, 4, 128], BF16, take(1024))
    Btok, u_Btok = C.sbu("Btok", [128, 4, 128], BF16, take(1024))
    HT, u_HT = C.sbu("HT", [128, 128], F32, take(512))
    HTb, u_HTb = C.sbu("HTb", [128, 128], BF16, take(256))
    LT = [C.sbu("LT%d" % h, [128, 128], F32, take(512)) for h in range(2)]
    Lm = [C.sbu("Lm%d" % h, [128, 128], F32, take(512)) for h in range(2)]
    MT = [C.sbu("MT%d" % h, [128, 128], BF16, take(256)) for h in range(2)]
    Ee = [C.sbu("Ee%d" % h, [128, 128], F32, take(512)) for h in range(2)]
    CE = [C.sbu("CE%d" % h, [128, 128], BF16, take(256)) for h in range(2)]
    xw, u_xw = C.sbu("xw", [128, 128], BF16, take(256))
    ysb, u_ysb = C.sbu("ysb", [128, 128], F32, take(512))
    ygo = [C.sbu("ygo%d" % i, [128, 512], F32, take(2048)) for i in range(2)]
    ygo_l = [S.lane("ygo%d" % i) for i in range(2)]
    Btab, u_Btab = C.sbu("Btab", [128, NQB * (NQB + 1) // 2], F32, take(8320))
    NPT = 4
    PT = [C.sbu("PT%d" % i, [128, 128], BF16, take(256)) for i in range(NPT)]
    rec, u_rec = C.sbu("rec", [128, 128], F32, take(512))
    ato = [C.sbu("ato%d" % i, [128, 512], F32, take(2048)) for i in range(2)]
    ato_l = [S.lane("ato%d" % i) for i in range(2)]
    assert o <= SB_TOP, o
    cl = S.lane("constA")
    cg = DmaGroup(cl)
    A("sp", lambda e: e.dma_start(out=ident[:], in_=T["ident"]), writes=[u_ident], group=cg)
    A("sp", lambda e: e.dma_start(out=maskF[:], in_=T["maskT"]), writes=[u_maskF], group=cg)
    A("sp", lambda e: e.dma_start(out=convw[:], in_=T["convw"]), writes=[u_convw], group=cg)
    A("sp", lambda e: e.dma_start(out=convb[:], in_=T["convb"]), writes=[u_convb], group=cg)
    A("sp", lambda e: e.dma_start(out=dskc[:], in_=T["dsk"]), writes=[u_dskc], group=cg)
    A("sp", lambda e: e.dma_start(out=bfc[:], in_=T["bf"]), writes=[u_bfc], group=cg)
    A("sp", lambda e: e.dma_start(out=dtb[:], in_=T["dtb"]), writes=[u_dtb], group=cg)
    A("sp", lambda e: e.dma_start(out=nega[:], in_=T["alog"]), writes=[u_nega], group=cg)
    for h in range(2):
        A("sp", lambda e, h=h: e.dma_start(out=sel[h][0][:], in_=T["sel"][h]), writes=[sel[h][1]], group=cg)
    A("pool", lambda e: e.dma_start(out=wA[:], in_=T["wA"]), writes=[u_wA], group=DmaGroup(S.lane("wA")))
    A("pool", lambda e: e.dma_start(out=wA2[:], in_=T["wA2"]), writes=[u_wA2], group=DmaGroup(S.lane("wA2")))
    A("dve", lambda e: e.tensor_copy(out=maskT[:], in_=maskF[:]), reads=[u_maskF], writes=[u_mask])
    A("dve", lambda e: e.memset(ones_bf[:], 1.0), writes=[u_ones])
    A("dve", lambda e: e.memset(ones_f[:], 1.0), writes=[u_onesf])
    A("dve", lambda e: e.memset(onesrow[:], 1.0), writes=[u_onesrow])
    A("dve", lambda e: e.memset(one1[:], 1.0), writes=[u_one1])
    A("dve", lambda e: e.memset(HT[:], 0.0), writes=[u_HT])
    A("dve", lambda e: e.memset(HTb[:], 0.0), writes=[u_HTb])
    A("dve", lambda e: e.memset(clast[:], 0.0), writes=[u_clast])
    A("dve", lambda e: e.memset(Frow[:], 0.0), writes=[u_Frow])
    for g in range(3):
        A("dve", lambda e, g=g: e.memset(cbuf[g][0][:], 0.0), writes=[cbuf[g][1]])
    A("act", lambda e: e.activation(out=nega[:], in_=nega[:], func=AF.Exp), reads=[u_nega], writes=[u_nega])
    A("dve", lambda e: e.tensor_scalar(out=nega[:], in0=nega[:], scalar1=-1.0, scalar2=None, op0=ALU.mult),
      reads=[u_nega], writes=[u_nega])
    A("dve", lambda e: e.tensor_scalar(out=bfc[:], in0=bfc[:], scalar1=-1.0, scalar2=None, op0=ALU.mult),
      reads=[u_bfc], writes=[u_bfc])

    hT_v = T["hT"].rearrange("(kc p) t -> p kc t", p=128)
    for sb in range(NSB):
        tsl = slice(sb * 512, (sb + 1) * 512)
        hpt, hpu = hp[sb % 2]
        grp = DmaGroup(hp_l[sb % 2])
        for k4 in range(4):
            A("pool", lambda e, k4=k4, hpt=hpt, tsl=tsl: e.dma_start(
                out=hpt[:, k4 * 4:(k4 + 1) * 4, :], in_=hT_v[:, k4 * 4:(k4 + 1) * 4, tsl]), writes=[hpu], group=grp)
        def proj(cb_lo, cb_n, wt, u_w, rows):
            bk, bu = C.next_bank()
            for kc in range(16):
                A("pe", lambda e, kc=kc, bk=bk, hpt=hpt: e.matmul(bk[0:rows, :], lhsT=wt[:, kc, cb_lo:cb_lo + cb_n], rhs=hpt[:, kc, :],
                                                      start=(kc == 0), stop=(kc == 15)), reads=[u_w, hpu], writes=[bu])
            return bk, bu
        bk, bu = proj(0, 128, wA, u_wA, 128)
        A("act", lambda e, bk=bk, tsl=tsl: e.copy(out=qT[:, tsl], in_=bk[:]), reads=[bu], writes=[u_qT])
        bk, bu = proj(128, 128, wA, u_wA, 128)
        A("dve", lambda e, bk=bk, tsl=tsl: e.tensor_copy(out=kT[:, tsl], in_=bk[:]), reads=[bu], writes=[u_kT])
        bk, bu = proj(256, 128, wA, u_wA, 128)
        A("act", lambda e, bk=bk: e.copy(out=vtmp[:], in_=bk[:]), reads=[bu], writes=[u_vtmp])
        vb, vbu = C.next_bank()
        for j in range(4):
            A("pe", lambda e, j=j, vb=vb: e.transpose(out=vb[:, j * 128:(j + 1) * 128], in_=vtmp[:, j * 128:(j + 1) * 128],
                                                     identity=ident[:]), reads=[u_vtmp, u_ident], writes=[vbu])
        A("act", lambda e, vb=vb, sb=sb: e.copy(out=Vt[:, sb * 4:(sb + 1) * 4, :],
                                                in_=vb[:].rearrange("p (j c) -> p j c", j=4)), reads=[vbu], writes=[u_Vt])
        bk, bu = proj(384, 128, wA, u_wA, 128)
        A("act", lambda e, bk=bk: e.activation(out=zsig[:], in_=bk[:], func=AF.Sigmoid), reads=[bu], writes=[u_zsig])
        A("dve", lambda e, bk=bk: e.tensor_tensor(out=zsT[:], in0=bk[:], in1=zsig[:], op=ALU.mult), reads=[bu, u_zsig], writes=[u_zsT])
        for g in range(3):
            bk, bu = proj(512 + 128 * g, 128, wA, u_wA, 128)
            cb, cbu = cbuf[g]
            ca, cau = cacc[g]
            cs, csu = csig[g]
            co, cou = cout[g]
            A("act", lambda e, bk=bk, cb=cb: e.copy(out=cb[:, 3:515], in_=bk[:]), reads=[bu], writes=[cbu])
            A("dve", lambda e, cb=cb, ca=ca, g=g: e.tensor_scalar(out=ca[:], in0=cb[:, 0:512], scalar1=convw[:, g, 0:1],
                                                                 scalar2=convb[:, g:g + 1], op0=ALU.mult, op1=ALU.add),
              reads=[cbu, u_convw, u_convb], writes=[cau])
            for k in range(1, 4):
                A("dve", lambda e, cb=cb, ca=ca, g=g, k=k: e.scalar_tensor_tensor(
                    out=ca[:], in0=cb[:, k:k + 512], scalar=convw[:, g, k:k + 1], in1=ca[:], op0=ALU.mult, op1=ALU.add),
                    reads=[cbu, u_convw, cau], writes=[cau])
            A("pool", lambda e, cb=cb: e.tensor_copy(out=cb[:, 0:3], in_=cb[:, 512:515]), reads=[cbu], writes=[cbu])
            A("act", lambda e, ca=ca, cs=cs: e.activation(out=cs[:], in_=ca[:], func=AF.Sigmoid), reads=[cau], writes=[csu])
            A("pool", lambda e, ca=ca, cs=cs, co=co: e.tensor_tensor(out=co[:], in0=ca[:], in1=cs[:], op=ALU.mult),
              reads=[cau, csu], writes=[cou])
        A("pool", lambda e: e.tensor_copy(out=BTb[:], in_=cout[1][0][:]), reads=[cout[1][1]], writes=[u_BTb])
        A("pool", lambda e: e.tensor_copy(out=CTb[:], in_=cout[2][0][:]), reads=[cout[2][1]], writes=[u_CTb])
        bk, bu = proj(0, 1, wA2, u_wA2, 1)
        A("act", lambda e, bk=bk: e.activation(out=frow[:], in_=bk[0:1, :], func=AF.Exp, bias=bfc[:], scale=-1.0),
          reads=[bu, u_bfc], writes=[u_frow])
        A("act", lambda e: e.activation(out=frow[:], in_=frow[:], func=AF.Ln, bias=one1[0:1, :], scale=1.0),
          reads=[u_frow, u_one1], writes=[u_frow])
        A("dve", lambda e: e.tensor_scalar(out=frow[:], in0=frow[:], scalar1=-1.0, scalar2=None, op0=ALU.mult),
          reads=[u_frow], writes=[u_frow])
        A("dve", lambda e: e.tensor_tensor_scan(out=crow[:], data0=onesrow[0:1, :], data1=frow[:], initial=clast[:],
                                                op0=ALU.mult, op1=ALU.add), reads=[u_frow, u_onesrow, u_clast], writes=[u_crow])
        A("dve", lambda e: e.tensor_copy(out=clast[:], in_=crow[:, 511:512]), reads=[u_crow], writes=[u_clast])
        A("dve", lambda e, sb=sb: e.tensor_copy(out=Frow[:, sb * 4 + 1:sb * 4 + 5], in_=crow[:, 127::128]), reads=[u_crow], writes=[u_Frow])
        cb_, cbu_ = C.next_bank()
        for j in range(4):
            A("pe", lambda e, j=j, cb_=cb_: e.matmul(cb_[:, j:j + 1], lhsT=crow[0:1, j * 128:(j + 1) * 128], rhs=ones_f[0:1, 0:1],
                                                    start=True, stop=True), reads=[u_crow, u_onesf], writes=[cbu_])
        A("dve", lambda e, cb_=cb_, sb=sb: e.tensor_copy(out=cT[:, sb * 4:(sb + 1) * 4], in_=cb_[:, 0:4]), reads=[cbu_], writes=[u_cT])
        bk, bu = proj(1, 2, wA2, u_wA2, 2)
        A("act", lambda e, bk=bk: e.activation(out=dtr[:], in_=bk[0:2, :], func=AF.Exp, bias=dtb[:], scale=1.0),
          reads=[bu, u_dtb], writes=[u_dtr])
        A("act", lambda e: e.activation(out=dtr[:], in_=dtr[:], func=AF.Ln, bias=one1[:], scale=1.0),
          reads=[u_dtr, u_one1], writes=[u_dtr])
        A("dve", lambda e: e.tensor_scalar(out=dAr[:], in0=dtr[:], scalar1=nega[:], scalar2=None, op0=ALU.mult),
          reads=[u_dtr, u_nega], writes=[u_dAr])
        for j in range(4):
            js = slice(j * 128, (j + 1) * 128)
            A("dve", lambda e, js=js: e.tensor_tensor_scan(out=acr[:, js], data0=onesrow[:, js], data1=dAr[:, js], initial=0.0,
                                                          op0=ALU.mult, op1=ALU.add), reads=[u_dAr, u_onesrow], writes=[u_acr])
        A("dve", lambda e: e.tensor_copy(out=alast[:], in_=acr[:, 127::128]), reads=[u_acr], writes=[u_alast])
        pc, pcu = C.next_bank()
        for j in range(4):
            js = slice(j * 128, (j + 1) * 128)
            A("pe", lambda e, j=j, js=js, pc=pc: e.matmul(pc[:, 2 * j:2 * j + 2], lhsT=acr[:, js], rhs=ident[0:2, 0:2],
                                                         start=True, stop=True), reads=[u_acr, u_ident], writes=[pcu])
            A("pe", lambda e, j=j, js=js, pc=pc: e.matmul(pc[:, 8 + 2 * j:8 + 2 * j + 2], lhsT=dtr[:, js], rhs=ident[0:2, 0:2],
                                                         start=True, stop=True), reads=[u_dtr, u_ident], writes=[pcu])
        for h in range(2):
            A("pe", lambda e, h=h, pc=pc: e.matmul(pc[:, 16 + 4 * h:16 + 4 * h + 4], lhsT=sel[h][0][:], rhs=alast[:],
                                                  start=True, stop=True), reads=[sel[h][1], u_alast], writes=[pcu])
        A("dve", lambda e, pc=pc: e.tensor_copy(out=colA[:].rearrange("p c h -> p (c h)"), in_=pc[:, 0:8]), reads=[pcu], writes=[u_colA])
        A("dve", lambda e, pc=pc: e.tensor_copy(out=colD[:].rearrange("p c h -> p (c h)"), in_=pc[:, 8:16]), reads=[pcu], writes=[u_colD])
        A("dve", lambda e, pc=pc: e.tensor_copy(out=colL[:].rearrange("p h c -> p (h c)"), in_=pc[:, 16:24]), reads=[pcu], writes=[u_colL])
        A("dve", lambda e: e.tensor_tensor(out=colW[:], in0=colL[:].rearrange("p h c -> p c h"), in1=colA[:], op=ALU.subtract),
          reads=[u_colL, u_colA], writes=[u_colW])
        A("act", lambda e: e.activation(out=colW[:], in_=colW[:], func=AF.Exp), reads=[u_colW], writes=[u_colW])
        A("dve", lambda e: e.tensor_tensor(out=colW[:], in0=colW[:], in1=colD[:], op=ALU.mult), reads=[u_colW, u_colD], writes=[u_colW])
        A("act", lambda e: e.activation(out=colE[:], in_=colL[:], func=AF.Exp), reads=[u_colL], writes=[u_colE])
        for src, dst, du in ((cout[0], xtok, u_xtok), (cout[1], Btok, u_Btok)):
            tb, tbu = C.next_bank()
            for j in range(4):
                A("pe", lambda e, j=j, tb=tb, src=src: e.transpose(out=tb[:, j * 128:(j + 1) * 128], in_=src[0][:, j * 128:(j + 1) * 128],
                                                                  identity=ident[:]), reads=[src[1], u_ident], writes=[tbu])
            A("act", lambda e, tb=tb, dst=dst: e.copy(out=dst[:], in_=tb[:].rearrange("p (j c) -> p j c", j=4)), reads=[tbu], writes=[du])
        yo, you = ygo[sb % 2]
        for j in range(4):
            js = slice(j * 128, (j + 1) * 128)
            cbk, cbku = C.next_bank()
            A("pe", lambda e, js=js, cbk=cbk: e.matmul(cbk[:, 0:128], lhsT=BTb[:, js], rhs=CTb[:, js], start=True, stop=True),
              reads=[u_BTb, u_CTb], writes=[cbku])
            ark, arku = C.next_bank()
            for h in range(2):
                A("pe", lambda e, h=h, js=js, ark=ark: e.matmul(ark[:, h * 128:(h + 1) * 128], lhsT=sel[h][0][:], rhs=acr[:, js],
                                                               start=True, stop=True), reads=[sel[h][1], u_acr], writes=[arku])
            for h in range(2):
                A("dve", lambda e, h=h, j=j: e.tensor_scalar(out=xw[:, h * 64:(h + 1) * 64], in0=xtok[:, j, h * 64:(h + 1) * 64],
                                                             scalar1=colW[:, j, h:h + 1], scalar2=None, op0=ALU.mult),
                  reads=[u_xtok, u_colW], writes=[u_xw])
            stk, stku = C.next_bank()
            A("pe", lambda e, j=j, stk=stk: e.matmul(stk[:, 0:128], lhsT=Btok[:, j, :], rhs=xw[:], start=True, stop=True),
              reads=[u_Btok, u_xw], writes=[stku])
            yk, yku = C.next_bank()
            for h in range(2):
                lt, ltu = LT[h]; lm, lmu = Lm[h]; mt, mtu = MT[h]; ee, eeu = Ee[h]; ce, ceu = CE[h]
                hs = slice(h * 64, (h + 1) * 64)
                A("dve", lambda e, h=h, j=j: e.tensor_scalar(out=colA[:, j, h:h + 1], in0=colA[:, j, h:h + 1], scalar1=-1.0, scalar2=None,
                                                             op0=ALU.mult), reads=[u_colA], writes=[u_colA])
                A("act", lambda e, h=h, j=j, ark=ark, lt=lt: e.activation(out=lt[:], in_=ark[:, h * 128:(h + 1) * 128], func=AF.Identity,
                                                                          bias=colA[:, j, h:h + 1], scale=1.0),
                  reads=[arku, u_colA], writes=[ltu])
                A("dve", lambda e, lt=lt: e.tensor_scalar(out=lt[:], in0=lt[:], scalar1=0.0, scalar2=None, op0=ALU.min),
                  reads=[ltu], writes=[ltu])
                A("act", lambda e, lt=lt: e.activation(out=lt[:], in_=lt[:], func=AF.Exp), reads=[ltu], writes=[ltu])
                A("dve", lambda e, h=h, j=j, lt=lt, lm=lm: e.scalar_tensor_tensor(out=lm[:], in0=lt[:], scalar=colD[:, j, h:h + 1], in1=maskF[:],
                                                                                  op0=ALU.mult, op1=ALU.mult),
                  reads=[ltu, u_colD, u_maskF], writes=[lmu])
                A("dve", lambda e, cbk=cbk, lm=lm, mt=mt: e.tensor_tensor(out=mt[:], in0=cbk[:, 0:128], in1=lm[:], op=ALU.mult),
                  reads=[cbku, lmu], writes=[mtu])
                A("act", lambda e, h=h, ark=ark, ee=ee: e.activation(out=ee[:], in_=ark[:, h * 128:(h + 1) * 128], func=AF.Exp),
                  reads=[arku], writes=[eeu])
                A("pool", lambda e, js=js, ee=ee, ce=ce: e.tensor_tensor(out=ce[:], in0=cout[2][0][:, js], in1=ee[:], op=ALU.mult),
                  reads=[cout[2][1], eeu], writes=[ceu])
                A("pe", lambda e, j=j, hs=hs, yk=yk, mt=mt: e.matmul(yk[hs, 0:128], lhsT=xtok[:, j, hs], rhs=mt[:], start=True, stop=False),
                  reads=[mtu, u_xtok], writes=[yku])
                A("pe", lambda e, hs=hs, yk=yk, ce=ce: e.matmul(yk[hs, 0:128], lhsT=HTb[:, hs], rhs=ce[:], start=False, stop=True),
                  reads=[ceu, u_HTb], writes=[yku])
            A("dve", lambda e, js=js, yk=yk: e.scalar_tensor_tensor(out=ysb[:], in0=cout[0][0][:, js], scalar=dskc[:, 0:1],
                                                                   in1=yk[:, 0:128], op0=ALU.mult, op1=ALU.add),
              reads=[yku, cout[0][1], u_dskc], writes=[u_ysb])
            A("pool", lambda e, js=js, yo=yo: e.tensor_tensor(out=yo[:, js], in0=ysb[:], in1=zsT[:, js], op=ALU.mult),
              reads=[u_ysb, u_zsT], writes=[you])
            for h in range(2):
                hs = slice(h * 64, (h + 1) * 64)
                A("dve", lambda e, h=h, j=j, hs=hs, stk=stk: e.scalar_tensor_tensor(out=HT[:, hs], in0=HT[:, hs], scalar=colE[:, h, j:j + 1],
                                                                                   in1=stk[:, hs], op0=ALU.mult, op1=ALU.add),
                  reads=[stku, u_HT, u_colE], writes=[u_HT])
            A("pool", lambda e: e.tensor_copy(out=HTb[:], in_=HT[:]), reads=[u_HT], writes=[u_HTb])
        A("sp", lambda e, yo=yo, tsl=tsl: e.dma_start(out=T["ygT"][:, tsl], in_=yo[:]), reads=[you], group=DmaGroup(ygo_l[sb % 2]))

    fk, fku = C.next_bank()
    A("pe", lambda e: e.matmul(fk[:, 0:NQB + 4], lhsT=ones_f[0:1, :], rhs=Frow[:], start=True, stop=True),
      reads=[u_onesf, u_Frow], writes=[fku])
    A("dve", lambda e: e.tensor_copy(out=Frep[:], in_=fk[:, 0:NQB + 4]), reads=[fku], writes=[u_Frep])
    offs = []
    oo = 0
    for i in range(NQB):
        offs.append(oo)
        A("dve", lambda e, i=i, oo=oo: e.tensor_scalar(out=Btab[:, oo:oo + i + 1], in0=cT[:, 0:i + 1], scalar1=-1.0, scalar2=Frep[:, i:i + 1],
                                                      op0=ALU.mult, op1=ALU.add), reads=[u_cT, u_Frep], writes=[u_Btab])
        oo += i + 1
    res = [C.reserve_bank() for _ in range(4)]
    pairs = [(i, j) for i in range(NQB) for j in range(i + 1)]
    sbanks = {}
    def issue_s(k):
        i, j = pairs[k]
        bk, bu = C.next_bank()
        sbanks[k] = (bk, bu)
        A("pe", lambda e, i=i, j=j, bk=bk: e.matmul(bk[:, 0:128], lhsT=kT[:, j * 128:(j + 1) * 128], rhs=qT[:, i * 128:(i + 1) * 128],
                                                    start=True, stop=True), reads=[u_kT, u_qT], writes=[bu])
    DEPTH_S = 2
    for k in range(min(DEPTH_S, len(pairs))):
        issue_s(k)
    for k, (i, j) in enumerate(pairs):
        if k + DEPTH_S < len(pairs):
            issue_s(k + DEPTH_S)
        bk, bu = sbanks.pop(k)
        pt, ptu = PT[k % NPT]
        (ab, abu) = res[(i % 2) * 2][1]
        (db, dbu) = res[(i % 2) * 2 + 1][1]
        col = offs[i] + j
        A("act", lambda e, bk=bk, pt=pt, col=col: e.activation(out=pt[:], in_=bk[:, 0:128], func=AF.Exp, bias=Btab[:, col:col + 1],
                                                              scale=ATT_SCALE), reads=[bu, u_Btab], writes=[ptu])
        if j == i:
            A("pool", lambda e, pt=pt: e.tensor_tensor(out=pt[:], in0=pt[:], in1=maskT[:], op=ALU.mult), reads=[ptu, u_mask], writes=[ptu])
        A("pe", lambda e, j=j, i=i, ab=ab, pt=pt: e.matmul(ab[:, 0:128], lhsT=Vt[:, j, :], rhs=pt[:], start=(j == 0), stop=(j == i)),
          reads=[u_Vt, ptu], writes=[abu])
        A("pe", lambda e, j=j, i=i, db=db, pt=pt: e.matmul(db[:, 0:128], lhsT=ones_bf[:], rhs=pt[:], start=(j == 0), stop=(j == i)),
          reads=[u_ones, ptu], writes=[dbu])
        if j == i:
            at, atu = ato[(i // 4) % 2]
            A("dve", lambda e, db=db: e.reciprocal(out=rec[:], in_=db[:, 0:128]), reads=[dbu], writes=[u_rec])
            A("dve", lambda e, ab=ab, at=at, i=i: e.tensor_tensor(out=at[:, (i % 4) * 128:(i % 4 + 1) * 128], in0=ab[:, 0:128], in1=rec[:],
                                                                  op=ALU.mult), reads=[abu, u_rec], writes=[atu])
            if i % 4 == 3:
                s4 = slice((i // 4) * 512, (i // 4 + 1) * 512)
                A("sp", lambda e, at=at, s4=s4: e.dma_start(out=T["attT"][:, s4], in_=at[:]), reads=[atu],
                  group=DmaGroup(ato_l[(i // 4) % 2]))
    for r in res:
        C.release_bank(r[0])


def stage_a_program():
    C = Ctx()
    T = {}
    T["hT"] = C.dram_in("hT", [D, SEQ])
    T["wA"] = C.dram_in("wA", [128, 16, 896])
    T["wA2"] = C.dram_in("wA2", [128, 16, 4])
    T["ident"] = C.dram_in("ident", [128, 128])
    T["maskT"] = C.dram_in("maskT", [128, 128])
    T["convw"] = C.dram_in("convw", [128, 3, 4])
    T["convb"] = C.dram_in("convb", [128, 3])
    T["dsk"] = C.dram_in("dsk", [128, 1])
    T["bf"] = C.dram_in("bf", [1, 1])
    T["dtb"] = C.dram_in("dtb", [2, 1])
    T["alog"] = C.dram_in("alog", [2, 1])
    T["sel"] = C.dram_in("sel", [2, 2, 128])
    T["attT"] = C.dram_out("attT", [128, SEQ])
    T["ygT"] = C.dram_out("ygT", [128, SEQ])
    build_stage_a(C, T)
    return C.finish()


def stage_a_inputs(inp, i, c):
    f = np.ascontiguousarray
    w_in = inp["w_in"][i]
    g = c // 2
    cols = []
    for base in (0, 1024, 2048):
        cols.append(np.arange(base + 128 * c, base + 128 * c + 128))
    cols.append(np.arange(3080 + 128 * c, 3080 + 128 * c + 128))
    cols.append(np.arange(4104 + 128 * c, 4104 + 128 * c + 128))
    cols.append(np.arange(4104 + 1024 + 128 * g, 4104 + 1024 + 128 * g + 128))
    cols.append(np.arange(4104 + 1536 + 128 * g, 4104 + 1536 + 128 * g + 128))
    cols = np.concatenate(cols)
    m = {}
    m["wA"] = f(w_in[:, cols].reshape(16, 128, 896).transpose(1, 0, 2))
    c2 = np.array([3072 + c, 6152 + 2 * c, 6152 + 2 * c + 1, 6152 + 2 * c + 1])
    m["wA2"] = f(w_in[:, c2].reshape(16, 128, 4).transpose(1, 0, 2))
    m["ident"] = np.eye(128, dtype=np.float32)
    m["maskT"] = np.triu(np.ones((128, 128), dtype=np.float32))
    ch = [np.arange(128 * c, 128 * c + 128), np.arange(1024 + 128 * g, 1024 + 128 * g + 128),
          np.arange(1536 + 128 * g, 1536 + 128 * g + 128)]
    m["convw"] = f(np.stack([inp["conv_w"][i][:, cc].T for cc in ch], axis=1))
    m["convb"] = f(np.stack([inp["conv_b"][i][cc] for cc in ch], axis=1))
    m["dsk"] = f(np.repeat(inp["d_skip"][i][2 * c:2 * c + 2], 64).reshape(128, 1))
    m["bf"] = f(inp["b_forget"][i][c:c + 1].reshape(1, 1))
    m["dtb"] = f(inp["dt_bias"][i][2 * c:2 * c + 2].reshape(2, 1))
    m["alog"] = f(inp["a_log"][i][2 * c:2 * c + 2].reshape(2, 1))
    sel = np.zeros((2, 2, 128), dtype=np.float32)
    sel[0, 0, :] = 1.0
    sel[1, 1, :] = 1.0
    m["sel"] = sel
    return m


def run_stage_a(nc, inp, i, h):
    hT = np.ascontiguousarray(h.T)
    in_maps = []
    for c in range(NCORES):
        m = stage_a_inputs(inp, i, c)
        m["hT"] = hT
        in_maps.append(m)
    res = run_bass_kernel_spmd(nc, in_maps, core_ids=list(range(NCORES)))
    attT = np.stack([r["attT"] for r in res.results])
    ygT = np.stack([r["ygT"] for r in res.results])
    return attT, ygT


def kernel(**inputs):
    inp = {k: np.asarray(v) for k, v in inputs.items()}
    h = np.ascontiguousarray(inp["x"][0], dtype=np.float32)
    ncA = stage_a_program()
    ncB = stage_b_program()
    for i in range(DEPTH):
        attT, ygT = run_stage_a(ncA, inp, i, h)
        mixedT = np.concatenate([attT.reshape(1024, SEQ), ygT.reshape(1024, SEQ)], axis=0)
        h = run_stage_b(ncB, inp, i, h, mixedT)
    return np.ascontiguousarray(h[None], dtype=np.float32)
```

```python
import numpy as np
import concourse.bass as bass
import concourse.mybir as mybir
from concourse.bass_utils import run_bass_kernel_spmd

F32 = mybir.dt.float32
BF16 = mybir.dt.bfloat16
AF = mybir.ActivationFunctionType
ALU = mybir.AluOpType
AX = mybir.AxisListType

NCORES = 8
D = 2048
SEQ = 8192
DEPTH = 2
TOK = SEQ // NCORES
NT = TOK // 128
NE = 16
DE = 1024
ALPHA = float((2 * DEPTH) ** 0.25)
LN_EPS = 1e-5
RMS_EPS = 1e-5
SB_BASE = 16512
SB_TOP = 229344
BIG = 1.0e4

ENGS = ("pe", "act", "dve", "pool", "sp")
SAME_ENG_SYNC = {"pe": False, "act": True, "dve": True, "pool": True, "sp": False}


class Unit:
    __slots__ = ("name", "space", "lo", "hi", "writer", "readers", "ovl")

    def __init__(self, name, space, lo, hi):
        self.name, self.space, self.lo, self.hi = name, space, lo, hi
        self.writer = None
        self.readers = []
        self.ovl = None


class Lane:
    def __init__(self, name):
        self.name = name
        self.sem = None
        self.count = 0


class DmaGroup:
    __slots__ = ("lane", "value")

    def __init__(self, lane):
        self.lane = lane
        self.value = 0


class Op:
    __slots__ = ("fn", "waits", "signals", "sigval", "group")

    def __init__(self, fn):
        self.fn = fn
        self.waits = []
        self.signals = False
        self.sigval = 0
        self.group = None


class Sched:
    def __init__(self):
        self.ops = {e: [] for e in ENGS}
        self.units = {"sb": [], "ps": [], "dr": []}
        self.lanes = []

    def unit(self, name, space, lo, hi):
        u = Unit(name, space, lo, hi)
        self.units[space].append(u)
        for v in self.units[space]:
            v.ovl = None
        return u

    def lane(self, name):
        l = Lane(name)
        self.lanes.append(l)
        return l

    def _ovl(self, u):
        if u.ovl is None:
            u.ovl = [v for v in self.units[u.space] if v.lo < u.hi and u.lo < v.hi]
        return u.ovl

    def add(self, eng, fn, reads=(), writes=(), group=None):
        op = Op(fn)
        idx = len(self.ops[eng])
        deps = []
        for u in reads:
            for v in self._ovl(u):
                if v.writer is not None:
                    deps.append(v.writer)
        for u in writes:
            for v in self._ovl(u):
                if v.writer is not None:
                    deps.append(v.writer)
                deps.extend(v.readers)
        if group is not None:
            myev = group
            group.lane.count += 16
            group.value = group.lane.count
            op.group = group
        else:
            myev = (eng, idx)
        seen = set()
        for d in deps:
            if d is myev:
                continue
            if isinstance(d, tuple):
                if d[0] == eng and not SAME_ENG_SYNC[eng]:
                    continue
                if d in seen:
                    continue
                seen.add(d)
                self.ops[d[0]][d[1]].signals = True
                op.waits.append(d)
            else:
                if id(d) in seen:
                    continue
                seen.add(id(d))
                op.waits.append(d)
        self.ops[eng].append(op)
        for u in writes:
            u.writer = myev
            u.readers = []
        for u in reads:
            if isinstance(myev, tuple):
                u.readers = [r for r in u.readers
                             if not (isinstance(r, tuple) and r[0] == eng)]
            u.readers.append(myev)
        return myev

    def emit(self, block, sems):
        for e in ENGS:
            c = 0
            for op in self.ops[e]:
                if op.signals:
                    c += 1
                    op.sigval = c
        final_lanes = [(l.sem, l.count) for l in self.lanes if l.count > 0]
        ops = self.ops

        def run(e, engobj):
            waited = {}
            for op in ops[e]:
                for d in op.waits:
                    if isinstance(d, tuple):
                        sem, val = sems[d[0]], ops[d[0]][d[1]].sigval
                    else:
                        sem, val = d.lane.sem, d.value
                    k = id(sem)
                    if waited.get(k, 0) >= val:
                        continue
                    waited[k] = val
                    engobj.wait_ge(sem, val)
                inst = op.fn(engobj)
                if op.group is not None:
                    inst.then_inc(op.group.lane.sem, 16)
                elif op.signals:
                    inst.then_inc(sems[e], 1)
            if e == "sp":
                for sem, cnt in final_lanes:
                    engobj.wait_ge(sem, cnt)

        @block.sync
        def _(eng):
            run("sp", eng)

        @block.tensor
        def _(eng):
            run("pe", eng)

        @block.scalar
        def _(eng):
            run("act", eng)

        @block.vector
        def _(eng):
            run("dve", eng)

        @block.gpsimd
        def _(eng):
            run("pool", eng)


class Ctx:
    def __init__(self):
        self.nc = bass.Bass("TRN2", target_bir_lowering=False)
        self.S = Sched()
        self.n_names = 0
        self.banks = []
        for b in range(8):
            t = self.nc.alloc_psum_tensor("psb%d" % b, [128, 512], F32)
            self.banks.append((t, self.S.unit("psb%d" % b, "ps", b * 2048, (b + 1) * 2048)))
        self.rot = list(range(8))
        self.bank_i = 0

    def dram_in(self, name, shape, dtype=F32):
        return self.nc.dram_tensor(name, list(shape), dtype, kind="ExternalInput").ap()

    def dram_out(self, name, shape, dtype=F32):
        return self.nc.dram_tensor(name, list(shape), dtype, kind="ExternalOutput").ap()

    def sb(self, name, shape, dtype, off):
        esz = 2 if dtype == BF16 else 4
        n = esz
        for s in shape[1:]:
            n *= s
        assert off >= SB_BASE and off + n <= SB_TOP, (name, off, n)
        self.n_names += 1
        t = self.nc.alloc_sbuf_tensor_at("%s_%d" % (name, self.n_names), list(shape), dtype, offset=off)
        return t, n

    def sbu(self, name, shape, dtype, off):
        t, n = self.sb(name, shape, dtype, off)
        return t, self.S.unit(name, "sb", off, off + n)

    def next_bank(self):
        b = self.banks[self.rot[self.bank_i % len(self.rot)]]
        self.bank_i += 1
        return b

    def reserve_bank(self):
        i = self.rot.pop()
        return i, self.banks[i]

    def release_bank(self, i):
        self.rot.append(i)

    def finish(self):
        nc = self.nc
        S = self.S
        sem_cms = [nc.semaphore("sem_" + e) for e in ENGS] + [nc.semaphore("lane_%d" % i) for i in range(len(S.lanes))]
        handles = [cm.__enter__() for cm in sem_cms]
        sems = dict(zip(ENGS, handles[:len(ENGS)]))
        for l, h in zip(S.lanes, handles[len(ENGS):]):
            l.sem = h
        with nc.Block() as block:
            S.emit(block, sems)
        return nc


def layer_norm_tile(C, x_ap, x_units, out_ap, out_units, g_t, b_t, gb_units, scr, scr_units, tag):
    S = C.S
    stats = scr[:, 0:24].rearrange("p (c s) -> p c s", c=4)
    for c in range(4):
        S.add("dve", lambda e, c=c: e.bn_stats(out=stats[:, c, :], in_=x_ap[:, c * 512:(c + 1) * 512]),
              reads=x_units, writes=scr_units)
    mv = scr[:, 24:26]
    S.add("dve", lambda e: e.bn_aggr(out=mv, in_=scr[:, 0:24]), reads=scr_units, writes=scr_units)
    rstd = scr[:, 26:27]
    S.add("act", lambda e: e.activation(out=rstd, in_=scr[:, 25:26], func=AF.Sqrt, bias=LN_EPS_AP[0], scale=1.0),
          reads=scr_units, writes=scr_units)
    S.add("dve", lambda e: e.reciprocal(out=rstd, in_=rstd), reads=scr_units, writes=scr_units)
    S.add("dve", lambda e: e.tensor_scalar(out=out_ap, in0=x_ap, scalar1=scr[:, 24:25], scalar2=rstd,
                                           op0=ALU.subtract, op1=ALU.mult),
          reads=x_units + scr_units, writes=out_units)
    S.add("pool", lambda e: e.tensor_tensor(out=out_ap, in0=out_ap, in1=g_t[:], op=ALU.mult),
          reads=out_units + gb_units, writes=out_units)
    S.add("pool", lambda e: e.tensor_tensor(out=out_ap, in0=out_ap, in1=b_t[:], op=ALU.add),
          reads=out_units + gb_units, writes=out_units)


LN_EPS_AP = [None]


def build_stage_b(C, T, stop=None, ne_run=NE):
    nc, S = C.nc, C.S
    o = SB_BASE
    X_OFF = o; o += 65536
    Y_OFF = o; o += 65536
    H1T_OFF = o; o += 32768
    HID_OFF = o; o += 16384
    M_OFF = o
    Xt, _ = C.sb("X", [128, NT, D], F32, X_OFF)
    xu = [[S.unit("X%d_%d" % (t, q), "sb", X_OFF + t * 8192 + q * 2048, X_OFF + t * 8192 + (q + 1) * 2048)
           for q in range(4)] for t in range(NT)]
    Yt, _ = C.sb("Y", [128, NT, D], F32, Y_OFF)
    yu = [[S.unit("Y%d_%d" % (t, q), "sb", Y_OFF + t * 8192 + q * 2048, Y_OFF + t * 8192 + (q + 1) * 2048)
           for q in range(4)] for t in range(NT)]
    mixT, u_mixT = C.sbu("mixT", [128, 16, TOK], BF16, Y_OFF)
    ygf, u_ygf = C.sbu("ygf", [128, 8, TOK], F32, Y_OFF + 32768)
    h1T, u_h1T = C.sbu("h1T", [128, 16, TOK], BF16, H1T_OFF)
    wo = [C.sbu("wo%d" % i, [128, 16, 512], BF16, H1T_OFF + i * 16384) for i in range(2)]
    hidT, u_hidT = C.sbu("hidT", [128, 8, TOK], BF16, HID_OFF)
    lng, u_lng = C.sbu("lng", [128, D], F32, HID_OFF)
    lnb, u_lnb = C.sbu("lnb", [128, D], F32, HID_OFF + 8192)
    m = M_OFF
    ident, u_ident = C.sbu("ident", [128, 128], F32, m); m += 512
    wr, u_wr = C.sbu("wr", [128, 16, 16], F32, m); m += 1024
    rb, u_rb = C.sbu("rb", [128, 16], F32, m); m += 64
    eps_t, u_eps = C.sbu("eps", [128, 1], F32, m); m += 32
    snw, u_snw = C.sbu("snw", [128, 8], F32, m); m += 32
    ones_f, u_onesf = C.sbu("ones_fB", [128, 128], F32, m); m += 512
    lnscr = [C.sbu("lnscr%d" % i, [128, 32], F32, m + 128 * i) for i in range(2)]; m += 256
    sc, u_sc = C.sbu("sc", [128, NT * 16], F32, m); m += 512
    bi, u_bi = C.sbu("bi", [128, NT * 16], F32, m); m += 512
    tm, u_tm = C.sbu("tm", [128, NT * 16], F32, m); m += 512
    comb, u_comb = C.sbu("comb", [128, NT * 16], F32, m); m += 512
    g4, u_g4 = C.sbu("g4", [128, 6, NT * 4], F32, m); m += 768
    g1, u_g1 = C.sbu("g1", [128, 2, NT], F32, m); m += 64
    h1f = [C.sbu("h1f%d" % i, [128, 4, 128], F32, m + 2048 * i) for i in range(2)]; m += 4096
    sig = [C.sbu("sig%d" % i, [128, 512], F32, m + 2048 * i) for i in range(2)]; m += 4096
    tmp = [C.sbu("tmp%d" % i, [128, 512], F32, m + 2048 * i) for i in range(2)]; m += 4096
    pT, u_pT = C.sbu("pT", [128, 2, TOK], BF16, m); m += 4096
    assert m <= SB_TOP, m
    NGU = 4
    gu = [C.sbu("gu%d" % i, [128, 2, 16, 128], BF16, X_OFF + i * 8192) for i in range(NGU)]
    NDN = 3
    dn = [C.sbu("dn%d" % i, [128, 8, 512], BF16, X_OFF + 32768 + i * 8192) for i in range(NDN)]
    wple, u_wple = C.sbu("wple", [128, 2, D], BF16, X_OFF + 57344)
    wpg = [C.sbu("wpg%d" % i, [128, 16, 512], BF16, X_OFF + i * 16384) for i in range(2)]
    gu_l = [S.lane("gu%d" % i) for i in range(NGU)]
    dn_l = [S.lane("dn%d" % i) for i in range(NDN)]
    wo_l = [S.lane("wo%d" % i) for i in range(2)]
    wpg_l = [S.lane("wpg%d" % i) for i in range(2)]
    c_l = S.lane("const")
    x_l = [S.lane("x%d" % t) for t in range(NT)]
    o_l = [S.lane("o%d" % t) for t in range(NT)]
    mix_l = S.lane("mix")

    cg = DmaGroup(c_l)
    S.add("sp", lambda e: e.dma_start(out=ident[:], in_=T["ident"]), writes=[u_ident], group=cg)
    S.add("sp", lambda e: e.dma_start(out=wr[:], in_=T["wr"]), writes=[u_wr], group=cg)
    S.add("sp", lambda e: e.dma_start(out=rb[:], in_=T["rbias"].broadcast_to([128, 16])), writes=[u_rb], group=cg)
    S.add("sp", lambda e: e.dma_start(out=lng[:], in_=T["ln1g"].broadcast_to([128, D])), writes=[u_lng], group=cg)
    S.add("sp", lambda e: e.dma_start(out=lnb[:], in_=T["ln1b"].broadcast_to([128, D])), writes=[u_lnb], group=cg)
    S.add("dve", lambda e: e.memset(eps_t[:], LN_EPS), writes=[u_eps])
    LN_EPS_AP[0] = eps_t[:]
    for t in range(NT):
        S.add("sp", lambda e, t=t: e.dma_start(out=Xt[:, t, :], in_=T["h"][t * 128:(t + 1) * 128, :]),
              writes=xu[t], group=DmaGroup(x_l[t]))
    mg = DmaGroup(mix_l)
    for c4 in range(2):
        S.add("pool", lambda e, c4=c4: e.dma_start(
            out=mixT[:, c4 * 4:(c4 + 1) * 4, :],
            in_=T["mixT"][c4 * 512:(c4 + 1) * 512, :].rearrange("(c p) t -> p c t", p=128)),
            writes=[u_mixT], group=mg)
    yg_g = DmaGroup(S.lane("ygf"))
    for c4 in range(2):
        S.add("sp", lambda e, c4=c4: e.dma_start(
            out=ygf[:, c4 * 4:(c4 + 1) * 4, :],
            in_=T["mixT"][1024 + c4 * 512:1024 + (c4 + 1) * 512, :].rearrange("(c p) t -> p c t", p=128)),
            writes=[u_ygf], group=yg_g)
    S.add("sp", lambda e: e.dma_start(out=snw[:], in_=T["snw"]), writes=[u_snw], group=cg)
    S.add("dve", lambda e: e.memset(ones_f[:], 1.0), writes=[u_onesf])
    for g in range(4):
        for th in range(2):
            ts_ = slice(th * 512, (th + 1) * 512)
            mbk, mbu = C.next_bank()
            for k in range(2):
                sq, squ = sig[k]
                S.add("act", lambda e, g=g, k=k, sq=sq, ts_=ts_: e.activation(out=sq[:], in_=ygf[:, 2 * g + k, ts_], func=AF.Square),
                      reads=[u_ygf], writes=[squ])
                S.add("pe", lambda e, k=k, sq=sq, mbk=mbk: e.matmul(mbk[:], lhsT=ones_f[:], rhs=sq[:], start=(k == 0), stop=(k == 1)),
                      reads=[u_onesf, squ], writes=[mbu])
            rs, rsu = tmp[0]
            S.add("act", lambda e, mbk=mbk, rs=rs: e.activation(out=rs[:], in_=mbk[:], func=AF.Sqrt, bias=eps_t[:], scale=1.0 / 256.0),
                  reads=[mbu, u_eps], writes=[rsu])
            S.add("dve", lambda e, rs=rs: e.reciprocal(out=rs[:], in_=rs[:]), reads=[rsu], writes=[rsu])
            for k in range(2):
                S.add("dve", lambda e, g=g, k=k, rs=rs, ts_=ts_: e.scalar_tensor_tensor(
                    out=mixT[:, 8 + 2 * g + k, ts_], in0=ygf[:, 2 * g + k, ts_], scalar=snw[:, 2 * g + k:2 * g + k + 1], in1=rs[:],
                    op0=ALU.mult, op1=ALU.mult), reads=[u_ygf, u_snw, rsu], writes=[u_mixT])
    S.add("pool", lambda e: e.dma_start(out=pT[:], in_=T["pT"].rearrange("(c p) t -> p c t", p=128)),
          writes=[u_pT], group=cg)

    for dq in range(4):
        wt, wu_ = wo[dq % 2]
        S.add("pool", lambda e, dq=dq, wt=wt: e.dma_start(out=wt[:], in_=T["wout"][dq]),
              writes=[wu_], group=DmaGroup(wo_l[dq % 2]))
        for t in range(NT):
            bk, bu = C.next_bank()
            for kc in range(16):
                S.add("pe", lambda e, kc=kc, t=t, bk=bk, wt=wt: e.matmul(
                    bk[:], lhsT=mixT[:, kc, t * 128:(t + 1) * 128], rhs=wt[:, kc, :],
                    start=(kc == 0), stop=(kc == 15)), reads=[u_mixT, wu_], writes=[bu])
            xs = Xt[:, t, dq * 512:(dq + 1) * 512]
            S.add("dve", lambda e, xs=xs, bk=bk: e.scalar_tensor_tensor(
                out=xs, in0=xs, scalar=ALPHA, in1=bk[:], op0=ALU.mult, op1=ALU.add),
                reads=[bu, xu[t][dq]], writes=[xu[t][dq]])

    def dump(src):
        for t in range(NT):
            S.add("sp", lambda e, t=t: e.dma_start(out=T["hout"][t * 128:(t + 1) * 128, :], in_=src[:, t, :]),
                  reads=xu[t] + yu[t], group=DmaGroup(o_l[t]))
    if stop == "p2a":
        return dump(Xt)
    rbi, (rbk, rbu) = C.reserve_bank()
    for t in range(NT):
        scr, scr_u = lnscr[t % 2]
        xt = Xt[:, t, :]
        layer_norm_tile(C, xt, xu[t], xt, xu[t], lng, lnb, [u_lng, u_lnb], scr, [scr_u], "ln1_%d" % t)
        if stop == "ln1":
            continue
        S.add("act", lambda e, t=t, xt=xt: e.activation(out=Yt[:, t, :], in_=xt, func=AF.Copy, scale=ALPHA),
              reads=xu[t], writes=yu[t])
        if stop == "acc":
            continue
        for kg in range(4):
            bk, bu = C.next_bank()
            for j in range(4):
                kc = kg * 4 + j
                S.add("pe", lambda e, j=j, kc=kc, bk=bk, xt=xt: e.transpose(
                    out=bk[:, j * 128:(j + 1) * 128], in_=xt[:, kc * 128:(kc + 1) * 128], identity=ident[:]),
                    reads=xu[t] + [u_ident], writes=[bu])
            hf, hfu = h1f[kg % 2]
            if stop == "tr0":
                S.add("act", lambda e, bk=bk, kg=kg, t=t: e.copy(out=Yt[:, t, kg * 512:(kg + 1) * 512], in_=bk[:]), reads=[bu], writes=yu[t])
                continue
            S.add("act", lambda e, bk=bk, kg=kg, t=t: e.copy(
                out=h1T[:, kg * 4:(kg + 1) * 4, t * 128:(t + 1) * 128],
                in_=bk[:].rearrange("p (j c) -> p j c", j=4)), reads=[bu], writes=[u_h1T])
            if stop == "tr1":
                continue
            S.add("act", lambda e, bk=bk, hf=hf: e.copy(
                out=hf[:].rearrange("p j c -> p (j c)"), in_=bk[:]), reads=[bu], writes=[hfu])
            if stop == "tr2":
                continue
            for j in range(4):
                kc = kg * 4 + j
                S.add("pe", lambda e, j=j, kc=kc, hf=hf, t=t: e.matmul(
                    rbk[:, t * 16:(t + 1) * 16], lhsT=hf[:, j, :], rhs=wr[:, kc, :],
                    start=(kc == 0), stop=(kc == 15)), reads=[hfu, u_wr], writes=[rbu])

    if stop in ("p2b", "ln1", "acc", "tr0", "tr1", "tr2"):
        return dump(Xt)
    NG = NT * 4
    def v3(ap):
        return ap.rearrange("p (g j) -> p g j", j=4)
    S.add("act", lambda e: e.activation(out=sc[:], in_=rbk[:, 0:NT * 16], func=AF.Sigmoid), reads=[rbu], writes=[u_sc])
    S.add("dve", lambda e: e.tensor_tensor(
        out=bi[:].rearrange("p (t x) -> p t x", x=16), in0=sc[:].rearrange("p (t x) -> p t x", x=16),
        in1=rb[:].rearrange("p (o x) -> p o x", o=1).broadcast_to([128, NT, 16]), op=ALU.add),
        reads=[u_sc, u_rb], writes=[u_bi])
    m1, m2, gs, gm, gt = (g4[:, i, :] for i in range(5))
    def bc(ap):
        return ap.rearrange("p (g o) -> p g o", o=1).broadcast_to([128, NG, 4])
    S.add("dve", lambda e: e.tensor_reduce(out=m1, in_=v3(bi[:]), axis=AX.X, op=ALU.max), reads=[u_bi], writes=[u_g4])
    S.add("dve", lambda e: e.tensor_tensor(out=v3(tm[:]), in0=v3(bi[:]), in1=bc(m1), op=ALU.is_ge), reads=[u_bi, u_g4], writes=[u_tm])
    S.add("dve", lambda e: e.scalar_tensor_tensor(out=tm[:], in0=tm[:], scalar=-BIG, in1=bi[:], op0=ALU.mult, op1=ALU.add),
          reads=[u_tm, u_bi], writes=[u_tm])
    S.add("dve", lambda e: e.tensor_reduce(out=m2, in_=v3(tm[:]), axis=AX.X, op=ALU.max), reads=[u_tm], writes=[u_g4])
    S.add("dve", lambda e: e.tensor_tensor(out=gs, in0=m1, in1=m2, op=ALU.add), reads=[u_g4], writes=[u_g4])
    gmax = g1[:, 0, :]
    S.add("dve", lambda e: e.tensor_reduce(out=gmax, in_=gs.rearrange("p (t g) -> p t g", g=4), axis=AX.X, op=ALU.max),
          reads=[u_g4], writes=[u_g1])
    S.add("dve", lambda e: e.tensor_tensor(
        out=gm.rearrange("p (t g) -> p t g", g=4), in0=gs.rearrange("p (t g) -> p t g", g=4),
        in1=gmax.rearrange("p (t o) -> p t o", o=1).broadcast_to([128, NT, 4]), op=ALU.is_ge),
        reads=[u_g4, u_g1], writes=[u_g4])
    S.add("dve", lambda e: e.tensor_tensor(out=v3(tm[:]), in0=v3(bi[:]), in1=bc(m2), op=ALU.is_ge), reads=[u_bi, u_g4], writes=[u_tm])
    S.add("dve", lambda e: e.tensor_tensor(out=v3(tm[:]), in0=v3(tm[:]), in1=bc(gm), op=ALU.mult), reads=[u_tm, u_g4], writes=[u_tm])
    S.add("dve", lambda e: e.tensor_tensor(out=tm[:], in0=tm[:], in1=sc[:], op=ALU.mult), reads=[u_tm, u_sc], writes=[u_tm])
    den = g1[:, 1, :]
    S.add("dve", lambda e: e.tensor_reduce(out=den, in_=tm[:].rearrange("p (t x) -> p t x", x=16), axis=AX.X, op=ALU.add),
          reads=[u_tm], writes=[u_g1])
    S.add("dve", lambda e: e.reciprocal(out=den, in_=den), reads=[u_g1], writes=[u_g1])
    S.add("dve", lambda e: e.tensor_tensor(
        out=comb[:].rearrange("p (t x) -> p t x", x=16), in0=tm[:].rearrange("p (t x) -> p t x", x=16),
        in1=den.rearrange("p (t o) -> p t o", o=1).broadcast_to([128, NT, 16]), op=ALU.mult),
        reads=[u_tm, u_g1], writes=[u_comb])

    C.release_bank(rbi)
    if stop == "route":
        S.add("dve", lambda e: e.tensor_copy(out=Yt[:, 0, 0:NT * 16], in_=comb[:]), reads=[u_comb], writes=yu[0])
        return dump(Yt)
    gu_i = 0
    dn_i = 0
    for ex in range(ne_run):
        for fc in range(8):
            gt_, gu_u = gu[gu_i % NGU]
            grp = DmaGroup(gu_l[gu_i % NGU])
            gu_i += 1
            S.add("pool", lambda e, ex=ex, fc=fc, gt_=gt_: e.dma_start(out=gt_[:, 0, :, :], in_=T["wg"][ex][fc]),
                  writes=[gu_u], group=grp)
            S.add("pool", lambda e, ex=ex, fc=fc, gt_=gt_: e.dma_start(out=gt_[:, 1, :, :], in_=T["wu"][ex][fc]),
                  writes=[gu_u], group=grp)
            bks = [C.next_bank() for _ in range(4)]
            for gi in range(2):
                for kc in range(16):
                    for th in range(2):
                        bk, bu = bks[gi * 2 + th]
                        S.add("pe", lambda e, gi=gi, kc=kc, th=th, bk=bk, gt_=gt_: e.matmul(
                            bk[:], lhsT=gt_[:, gi, kc, :], rhs=h1T[:, kc, th * 512:(th + 1) * 512],
                            start=(kc == 0), stop=(kc == 15)), reads=[gu_u, u_h1T], writes=[bu])
            for th in range(2):
                gbk, gbu = bks[th]
                ubk, ubu = bks[2 + th]
                sg, sgu = sig[th]
                tp, tpu = tmp[th]
                S.add("act", lambda e, gbk=gbk, sg=sg: e.activation(out=sg[:], in_=gbk[:], func=AF.Sigmoid),
                      reads=[gbu], writes=[sgu])
                S.add("dve", lambda e, gbk=gbk, sg=sg, tp=tp: e.tensor_tensor(out=tp[:], in0=gbk[:], in1=sg[:], op=ALU.mult),
                      reads=[gbu, sgu], writes=[tpu])
                S.add("dve", lambda e, ubk=ubk, tp=tp, fc=fc, th=th: e.tensor_tensor(
                    out=hidT[:, fc, th * 512:(th + 1) * 512], in0=ubk[:], in1=tp[:], op=ALU.mult),
                    reads=[ubu, tpu], writes=[u_hidT])
        for dq in range(4):
            dt_, dn_u = dn[dn_i % NDN]
            grp = DmaGroup(dn_l[dn_i % NDN])
            dn_i += 1
            S.add("pool", lambda e, ex=ex, dq=dq, dt_=dt_: e.dma_start(out=dt_[:], in_=T["wd"][ex][dq]),
                  writes=[dn_u], group=grp)
            for t in range(NT):
                bk, bu = C.next_bank()
                for fc in range(8):
                    S.add("pe", lambda e, fc=fc, t=t, bk=bk, dt_=dt_: e.matmul(
                        bk[:], lhsT=hidT[:, fc, t * 128:(t + 1) * 128], rhs=dt_[:, fc, :],
                        start=(fc == 0), stop=(fc == 7)), reads=[u_hidT, dn_u], writes=[bu])
                ys = Yt[:, t, dq * 512:(dq + 1) * 512]
                S.add("dve", lambda e, ys=ys, bk=bk, t=t, ex=ex: e.scalar_tensor_tensor(
                    out=ys, in0=bk[:], scalar=comb[:, t * 16 + ex:t * 16 + ex + 1], in1=ys, op0=ALU.mult, op1=ALU.add),
                    reads=[bu, u_comb, yu[t][dq]], writes=[yu[t][dq]])

    if stop == "moe":
        return dump(Yt)
    S.add("pool", lambda e: e.dma_start(out=wple[:], in_=T["wple"]), writes=[u_wple], group=DmaGroup(S.lane("wple")))
    S.add("sp", lambda e: e.dma_start(out=lng[:], in_=T["ln2g"].broadcast_to([128, D])), writes=[u_lng], group=DmaGroup(S.lane("ln2g")))
    S.add("sp", lambda e: e.dma_start(out=lnb[:], in_=T["ln2b"].broadcast_to([128, D])), writes=[u_lnb], group=DmaGroup(S.lane("ln2b")))
    for dq in range(4):
        wt, wu_ = wpg[dq % 2]
        S.add("pool", lambda e, dq=dq, wt=wt: e.dma_start(out=wt[:], in_=T["wpg"][dq]),
              writes=[wu_], group=DmaGroup(wpg_l[dq % 2]))
        for t in range(NT):
            gbk, gbu = C.next_bank()
            pbk, pbu = C.next_bank()
            for kc in range(16):
                S.add("pe", lambda e, kc=kc, t=t, gbk=gbk, wt=wt: e.matmul(
                    gbk[:], lhsT=h1T[:, kc, t * 128:(t + 1) * 128], rhs=wt[:, kc, :],
                    start=(kc == 0), stop=(kc == 15)), reads=[u_h1T, wu_], writes=[gbu])
            for kc in range(2):
                S.add("pe", lambda e, kc=kc, t=t, pbk=pbk, dq=dq: e.matmul(
                    pbk[:], lhsT=pT[:, kc, t * 128:(t + 1) * 128], rhs=wple[:, kc, dq * 512:(dq + 1) * 512],
                    start=(kc == 0), stop=(kc == 1)), reads=[u_pT, u_wple], writes=[pbu])
            sg, sgu = sig[t % 2]
            tp, tpu = tmp[t % 2]
            S.add("act", lambda e, gbk=gbk, sg=sg: e.activation(out=sg[:], in_=gbk[:], func=AF.Sigmoid),
                  reads=[gbu], writes=[sgu])
            S.add("dve", lambda e, pbk=pbk, sg=sg, tp=tp: e.tensor_tensor(out=tp[:], in0=pbk[:], in1=sg[:], op=ALU.mult),
                  reads=[pbu, sgu], writes=[tpu])
            ys = Yt[:, t, dq * 512:(dq + 1) * 512]
            S.add("pool", lambda e, ys=ys, tp=tp: e.tensor_tensor(out=ys, in0=ys, in1=tp[:], op=ALU.add),
                  reads=[tpu, yu[t][dq]], writes=[yu[t][dq]])

    if stop == "ple":
        return dump(Yt)
    for t in range(NT):
        scr, scr_u = lnscr[t % 2]
        yt = Yt[:, t, :]
        layer_norm_tile(C, yt, yu[t], yt, yu[t], lng, lnb, [u_lng, u_lnb], scr, [scr_u], "ln2_%d" % t)
        S.add("sp", lambda e, t=t, yt=yt: e.dma_start(out=T["hout"][t * 128:(t + 1) * 128, :], in_=yt),
              reads=yu[t], group=DmaGroup(o_l[t]))


def stage_b_program(stop=None, ne_run=NE):
    C = Ctx()
    T = {}
    T["h"] = C.dram_in("h", [TOK, D])
    T["mixT"] = C.dram_in("mixT", [D, TOK])
    T["pT"] = C.dram_in("pT", [256, TOK])
    T["wout"] = C.dram_in("wout", [4, 128, 16, 512])
    T["ln1g"] = C.dram_in("ln1g", [1, D]); T["ln1b"] = C.dram_in("ln1b", [1, D])
    T["ln2g"] = C.dram_in("ln2g", [1, D]); T["ln2b"] = C.dram_in("ln2b", [1, D])
    T["wr"] = C.dram_in("wr", [128, 16, 16])
    T["rbias"] = C.dram_in("rbias", [1, 16])
    ne = ne_run if stop in (None, "moe", "ple") else 1
    for k, shp in (("wg", [8, 128, 16, 128]), ("wu", [8, 128, 16, 128]), ("wd", [4, 128, 8, 512])):
        T[k] = [C.dram_in("%s%d" % (k, ex), shp) for ex in range(ne)]
    T["wpg"] = C.dram_in("wpg", [4, 128, 16, 512])
    T["wple"] = C.dram_in("wple", [128, 2, D])
    T["ident"] = C.dram_in("ident", [128, 128])
    T["snw"] = C.dram_in("snw", [128, 8])
    T["hout"] = C.dram_out("hout", [TOK, D])
    build_stage_b(C, T, stop, ne_run)
    return C.finish()


def stage_b_weights(inp, i):
    f = np.ascontiguousarray
    W = {}
    W["wout"] = f(inp["w_out"][i].reshape(16, 128, 4, 512).transpose(2, 1, 0, 3))
    W["ln1g"] = f(inp["ln1_g"][i].reshape(1, D)); W["ln1b"] = f(inp["ln1_b"][i].reshape(1, D))
    W["ln2g"] = f(inp["ln2_g"][i].reshape(1, D)); W["ln2b"] = f(inp["ln2_b"][i].reshape(1, D))
    W["wr"] = f(inp["w_router"].reshape(16, 128, 16).transpose(1, 0, 2))
    W["rbias"] = f(inp["router_bias"].reshape(1, 16))
    W["wg"] = f(inp["w_gate"][i].reshape(NE, 16, 128, 8, 128).transpose(0, 3, 2, 1, 4))
    W["wu"] = f(inp["w_up"][i].reshape(NE, 16, 128, 8, 128).transpose(0, 3, 2, 1, 4))
    W["wd"] = f(inp["w_down"][i].reshape(NE, 8, 128, 4, 512).transpose(0, 3, 2, 1, 4))
    W["wpg"] = f(inp["w_ple_gate"][i].reshape(16, 128, 4, 512).transpose(2, 1, 0, 3))
    W["wple"] = f(inp["w_ple"][i].reshape(2, 128, D).transpose(1, 0, 2))
    W["ident"] = np.eye(128, dtype=np.float32)
    W["snw"] = f(inp["ssd_norm_w"][i].reshape(8, 128).T)
    return W


def run_stage_b(nc, inp, i, h, mixedT, ne=NE):
    W = stage_b_weights(inp, i)
    for k in ("wg", "wu", "wd"):
        big = W.pop(k)
        for ex in range(ne):
            W["%s%d" % (k, ex)] = np.ascontiguousarray(big[ex])
    p = inp["p"][i, 0]
    in_maps = []
    for c in range(NCORES):
        sl = slice(c * TOK, (c + 1) * TOK)
        m = dict(W)
        m["h"] = np.ascontiguousarray(h[sl])
        m["mixT"] = np.ascontiguousarray(mixedT[:, sl])
        m["pT"] = np.ascontiguousarray(p[sl].T)
        in_maps.append(m)
    res = run_bass_kernel_spmd(nc, in_maps, core_ids=list(range(NCORES)))
    return np.concatenate([r["hout"] for r in res.results], axis=0)


NSB = SEQ // 512
NQB = SEQ // 128
ATT_SCALE = float(128 ** -0.5)


def build_stage_a(C, T):
    nc, S = C.nc, C.S
    A = S.add
    o = SB_BASE
    def take(n):
        nonlocal o
        r = o
        o += n
        return r
    qT, u_qT = C.sbu("qT", [128, SEQ], BF16, take(16384))
    kT, u_kT = C.sbu("kT", [128, SEQ], BF16, take(16384))
    Vt, u_Vt = C.sbu("Vt", [128, NQB, 128], BF16, take(16384))
    wA, u_wA = C.sbu("wA", [128, 16, 896], BF16, take(28672))
    wA2, u_wA2 = C.sbu("wA2", [128, 16, 4], BF16, take(128))
    hp = [C.sbu("hp%d" % i, [128, 16, 512], BF16, take(16384)) for i in range(2)]
    hp_l = [S.lane("hp%d" % i) for i in range(2)]
    cbuf = [C.sbu("cb%d" % g, [128, 516], F32, take(2080)) for g in range(3)]
    ident, u_ident = C.sbu("identA", [128, 128], F32, take(512))
    maskT, u_mask = C.sbu("maskT", [128, 128], BF16, take(256))
    maskF, u_maskF = C.sbu("maskF", [128, 128], F32, take(512))
    ones_bf, u_ones = C.sbu("ones_bf", [128, 128], BF16, take(256))
    ones_f, u_onesf = C.sbu("ones_f", [128, 128], F32, take(512))
    sel = [C.sbu("sel%d" % h, [2, 128], F32, take(512)) for h in range(2)]
    convw, u_convw = C.sbu("convw", [128, 3, 4], F32, take(64))
    convb, u_convb = C.sbu("convb", [128, 3], F32, take(32))
    dskc, u_dskc = C.sbu("dskc", [128, 1], F32, take(32))
    bfc, u_bfc = C.sbu("bfc", [1, 1], F32, take(32))
    dtb, u_dtb = C.sbu("dtb", [2, 1], F32, take(32))
    nega, u_nega = C.sbu("nega", [2, 1], F32, take(32))
    one1, u_one1 = C.sbu("one1", [2, 1], F32, take(32))
    cT, u_cT = C.sbu("cT", [128, NQB], F32, take(256))
    Frow, u_Frow = C.sbu("Frow", [1, NQB + 4], F32, take(288))
    Frep, u_Frep = C.sbu("Frep", [128, NQB + 4], F32, take(288))
    clast, u_clast = C.sbu("clast", [1, 1], F32, take(32))
    frow, u_frow = C.sbu("frow", [1, 512], F32, take(2048))
    crow, u_crow = C.sbu("crow", [1, 512], F32, take(2048))
    dtr, u_dtr = C.sbu("dtr", [2, 512], F32, take(2048))
    dAr, u_dAr = C.sbu("dAr", [2, 512], F32, take(2048))
    acr, u_acr = C.sbu("acr", [2, 512], F32, take(2048))
    onesrow, u_onesrow = C.sbu("onesrow", [2, 512], F32, take(2048))
    alast, u_alast = C.sbu("alast", [2, 4], F32, take(32))
    colA, u_colA = C.sbu("colA", [128, 4, 2], F32, take(32))
    colD, u_colD = C.sbu("colD", [128, 4, 2], F32, take(32))
    colL, u_colL = C.sbu("colL", [128, 2, 4], F32, take(32))
    colW, u_colW = C.sbu("colW", [128, 4, 2], F32, take(32))
    colE, u_colE = C.sbu("colE", [128, 2, 4], F32, take(32))
    vtmp, u_vtmp = C.sbu("vtmp", [128, 512], F32, take(2048))
    zsig, u_zsig = C.sbu("zsig", [128, 512], F32, take(2048))
    zsT, u_zsT = C.sbu("zsT", [128, 512], F32, take(2048))
    cacc = [C.sbu("cacc%d" % g, [128, 512], F32, take(2048)) for g in range(3)]
    csig = [C.sbu("csig%d" % g, [128, 512], F32, take(2048)) for g in range(3)]
    cout = [C.sbu("cout%d" % g, [128, 512], F32, take(2048)) for g in range(3)]
    BTb, u_BTb = C.sbu("BTb", [128, 512], BF16, take(1024))
    CTb, u_CTb = C.sbu("CTb", [128, 512], BF16, take(1024))
    xtok, u_xtok = C.sbu("xtok", [128, 4, 128], BF16, take(1024))
    Btok, u_Btok = C.sbu("Btok", [128, 4, 128], BF16, take(1024))
    HT, u_HT = C.sbu("HT", [128, 128], F32, take(512))
    HTb, u_HTb = C.sbu("HTb", [128, 128], BF16, take(256))
    LT = [C.sbu("LT%d" % h, [128, 128], F32, take(512)) for h in range(2)]
    Lm = [C.sbu("Lm%d" % h, [128, 128], F32, take(512)) for h in range(2)]
    MT = [C.sbu("MT%d" % h, [128, 128], BF16, take(256)) for h in range(2)]
    Ee = [C.sbu("Ee%d" % h, [128, 128], F32, take(512)) for h in range(2)]
    CE = [C.sbu("CE%d" % h, [128, 128], BF16, take(256)) for h in range(2)]
    xw, u_xw = C.sbu("xw", [128, 128], BF16, take(256))
    ysb, u_ysb = C.sbu("ysb", [128, 128], F32, take(512))
    ygo = [C.sbu("ygo%d" % i, [128, 512], F32, take(2048)) for i in range(2)]
    ygo_l = [S.lane("ygo%d" % i) for i in range(2)]
    Btab, u_Btab = C.sbu("Btab", [128, NQB * (NQB + 1) // 2], F32, take(8320))
    NPT = 4
    PT = [C.sbu("PT%d" % i, [128, 128], BF16, take(256)) for i in range(NPT)]
    rec, u_rec = C.sbu("rec", [128, 128], F32, take(512))
    ato = [C.sbu("ato%d" % i, [128, 512], F32, take(2048)) for i in range(2)]
    ato_l = [S.lane("ato%d" % i) for i in range(2)]
    assert o <= SB_TOP, o
    cl = S.lane("constA")
    cg = DmaGroup(cl)
    A("sp", lambda e: e.dma_start(out=ident[:], in_=T["ident"]), writes=[u_ident], group=cg)
    A("sp", lambda e: e.dma_start(out=maskF[:], in_=T["maskT"]), writes=[u_maskF], group=cg)
    A("sp", lambda e: e.dma_start(out=convw[:], in_=T["convw"]), writes=[u_convw], group=cg)
    A("sp", lambda e: e.dma_start(out=convb[:], in_=T["convb"]), writes=[u_convb], group=cg)
    A("sp", lambda e: e.dma_start(out=dskc[:], in_=T["dsk"]), writes=[u_dskc], group=cg)
    A("sp", lambda e: e.dma_start(out=bfc[:], in_=T["bf"]), writes=[u_bfc], group=cg)
    A("sp", lambda e: e.dma_start(out=dtb[:], in_=T["dtb"]), writes=[u_dtb], group=cg)
    A("sp", lambda e: e.dma_start(out=nega[:], in_=T["alog"]), writes=[u_nega], group=cg)
    for h in range(2):
        A("sp", lambda e, h=h: e.dma_start(out=sel[h][0][:], in_=T["sel"][h]), writes=[sel[h][1]], group=cg)
    A("pool", lambda e: e.dma_start(out=wA[:], in_=T["wA"]), writes=[u_wA], group=DmaGroup(S.lane("wA")))
    A("pool", lambda e: e.dma_start(out=wA2[:], in_=T["wA2"]), writes=[u_wA2], group=DmaGroup(S.lane("wA2")))
    A("dve", lambda e: e.tensor_copy(out=maskT[:], in_=maskF[:]), reads=[u_maskF], writes=[u_mask])
    A("dve", lambda e: e.memset(ones_bf[:], 1.0), writes=[u_ones])
    A("dve", lambda e: e.memset(ones_f[:], 1.0), writes=[u_onesf])
    A("dve", lambda e: e.memset(onesrow[:], 1.0), writes=[u_onesrow])
    A("dve", lambda e: e.memset(one1[:], 1.0), writes=[u_one1])
    A("dve", lambda e: e.memset(HT[:], 0.0), writes=[u_HT])
    A("dve", lambda e: e.memset(HTb[:], 0.0), writes=[u_HTb])
    A("dve", lambda e: e.memset(clast[:], 0.0), writes=[u_clast])
    A("dve", lambda e: e.memset(Frow[:], 0.0), writes=[u_Frow])
    for g in range(3):
        A("dve", lambda e, g=g: e.memset(cbuf[g][0][:], 0.0), writes=[cbuf[g][1]])
    A("act", lambda e: e.activation(out=nega[:], in_=nega[:], func=AF.Exp), reads=[u_nega], writes=[u_nega])
    A("dve", lambda e: e.tensor_scalar(out=nega[:], in0=nega[:], scalar1=-1.0, scalar2=None, op0=ALU.mult),
      reads=[u_nega], writes=[u_nega])
    A("dve", lambda e: e.tensor_scalar(out=bfc[:], in0=bfc[:], scalar1=-1.0, scalar2=None, op0=ALU.mult),
      reads=[u_bfc], writes=[u_bfc])

    hT_v = T["hT"].rearrange("(kc p) t -> p kc t", p=128)
    for sb in range(NSB):
        tsl = slice(sb * 512, (sb + 1) * 512)
        hpt, hpu = hp[sb % 2]
        grp = DmaGroup(hp_l[sb % 2])
        for k4 in range(4):
            A("pool", lambda e, k4=k4, hpt=hpt, tsl=tsl: e.dma_start(
                out=hpt[:, k4 * 4:(k4 + 1) * 4, :], in_=hT_v[:, k4 * 4:(k4 + 1) * 4, tsl]), writes=[hpu], group=grp)
        def proj(cb_lo, cb_n, wt, u_w, rows):
            bk, bu = C.next_bank()
            for kc in range(16):
                A("pe", lambda e, kc=kc, bk=bk, hpt=hpt: e.matmul(bk[0:rows, :], lhsT=wt[:, kc, cb_lo:cb_lo + cb_n], rhs=hpt[:, kc, :],
                                                      start=(kc == 0), stop=(kc == 15)), reads=[u_w, hpu], writes=[bu])
            return bk, bu
        bk, bu = proj(0, 128, wA, u_wA, 128)
        A("act", lambda e, bk=bk, tsl=tsl: e.copy(out=qT[:, tsl], in_=bk[:]), reads=[bu], writes=[u_qT])
        bk, bu = proj(128, 128, wA, u_wA, 128)
        A("dve", lambda e, bk=bk, tsl=tsl: e.tensor_copy(out=kT[:, tsl], in_=bk[:]), reads=[bu], writes=[u_kT])
        bk, bu = proj(256, 128, wA, u_wA, 128)
        A("act", lambda e, bk=bk: e.copy(out=vtmp[:], in_=bk[:]), reads=[bu], writes=[u_vtmp])
        vb, vbu = C.next_bank()
        for j in range(4):
            A("pe", lambda e, j=j, vb=vb: e.transpose(out=vb[:, j * 128:(j + 1) * 128], in_=vtmp[:, j * 128:(j + 1) * 128],
                                                     identity=ident[:]), reads=[u_vtmp, u_ident], writes=[vbu])
        A("act", lambda e, vb=vb, sb=sb: e.copy(out=Vt[:, sb * 4:(sb + 1) * 4, :],
                                                in_=vb[:].rearrange("p (j c) -> p j c", j=4)), reads=[vbu], writes=[u_Vt])
        bk, bu = proj(384, 128, wA, u_wA, 128)
        A("act", lambda e, bk=bk: e.activation(out=zsig[:], in_=bk[:], func=AF.Sigmoid), reads=[bu], writes=[u_zsig])
        A("dve", lambda e, bk=bk: e.tensor_tensor(out=zsT[:], in0=bk[:], in1=zsig[:], op=ALU.mult), reads=[bu, u_zsig], writes=[u_zsT])
        for g in range(3):
            bk, bu = proj(512 + 128 * g, 128, wA, u_wA, 128)
            cb, cbu = cbuf[g]
            ca, cau = cacc[g]
            cs, csu = csig[g]
            co, cou = cout[g]
            A("act", lambda e, bk=bk, cb=cb: e.copy(out=cb[:, 3:515], in_=bk[:]), reads=[bu], writes=[cbu])
            A("dve", lambda e, cb=cb, ca=ca, g=g: e.tensor_scalar(out=ca[:], in0=cb[:, 0:512], scalar1=convw[:, g, 0:1],
                                                                 scalar2=convb[:, g:g + 1], op0=ALU.mult, op1=ALU.add),
              reads=[cbu, u_convw, u_convb], writes=[cau])
            for k in range(1, 4):
                A("dve", lambda e, cb=cb, ca=ca, g=g, k=k: e.scalar_tensor_tensor(
                    out=ca[:], in0=cb[:, k:k + 512], scalar=convw[:, g, k:k + 1], in1=ca[:], op0=ALU.mult, op1=ALU.add),
                    reads=[cbu, u_convw, cau], writes=[cau])
            A("pool", lambda e, cb=cb: e.tensor_copy(out=cb[:, 0:3], in_=cb[:, 512:515]), reads=[cbu], writes=[cbu])
            A("act", lambda e, ca=ca, cs=cs: e.activation(out=cs[:], in_=ca[:], func=AF.Sigmoid), reads=[cau], writes=[csu])
            A("pool", lambda e, ca=ca, cs=cs, co=co: e.tensor_tensor(out=co[:], in0=ca[:], in1=cs[:], op=ALU.mult),
              reads=[cau, csu], writes=[cou])
        A("pool", lambda e: e.tensor_copy(out=BTb[:], in_=cout[1][0][:]), reads=[cout[1][1]], writes=[u_BTb])
        A("pool", lambda e: e.tensor_copy(out=CTb[:], in_=cout[2][0][:]), reads=[cout[2][1]], writes=[u_CTb])
        bk, bu = proj(0, 1, wA2, u_wA2, 1)
        A("act", lambda e, bk=bk: e.activation(out=frow[:], in_=bk[0:1, :], func=AF.Exp, bias=bfc[:], scale=-1.0),
          reads=[bu, u_bfc], writes=[u_frow])
        A("act", lambda e: e.activation(out=frow[:], in_=frow[:], func=AF.Ln, bias=one1[0:1, :], scale=1.0),
          reads=[u_frow, u_one1], writes=[u_frow])
        A("dve", lambda e: e.tensor_scalar(out=frow[:], in0=frow[:], scalar1=-1.0, scalar2=None, op0=ALU.mult),
          reads=[u_frow], writes=[u_frow])
        A("dve", lambda e: e.tensor_tensor_scan(out=crow[:], data0=onesrow[0:1, :], data1=frow[:], initial=clast[:],
                                                op0=ALU.mult, op1=ALU.add), reads=[u_frow, u_onesrow, u_clast], writes=[u_crow])
        A("dve", lambda e: e.tensor_copy(out=clast[:], in_=crow[:, 511:512]), reads=[u_crow], writes=[u_clast])
        A("dve", lambda e, sb=sb: e.tensor_copy(out=Frow[:, sb * 4 + 1:sb * 4 + 5], in_=crow[:, 127::128]), reads=[u_crow], writes=[u_Frow])
        cb_, cbu_ = C.next_bank()
        for j in range(4):
            A("pe", lambda e, j=j, cb_=cb_: e.matmul(cb_[:, j:j + 1], lhsT=crow[0:1, j * 128:(j + 1) * 128], rhs=ones_f[0:1, 0:1],
                                                    start=True, stop=True), reads=[u_crow, u_onesf], writes=[cbu_])
        A("dve", lambda e, cb_=cb_, sb=sb: e.tensor_copy(out=cT[:, sb * 4:(sb + 1) * 4], in_=cb_[:, 0:4]), reads=[cbu_], writes=[u_cT])
        bk, bu = proj(1, 2, wA2, u_wA2, 2)
        A("act", lambda e, bk=bk: e.activation(out=dtr[:], in_=bk[0:2, :], func=AF.Exp, bias=dtb[:], scale=1.0),
          reads=[bu, u_dtb], writes=[u_dtr])
        A("act", lambda e: e.activation(out=dtr[:], in_=dtr[:], func=AF.Ln, bias=one1[:], scale=1.0),
          reads=[u_dtr, u_one1], writes=[u_dtr])
        A("dve", lambda e: e.tensor_scalar(out=dAr[:], in0=dtr[:], scalar1=nega[:], scalar2=None, op0=ALU.mult),
          reads=[u_dtr, u_nega], writes=[u_dAr])
        for j in range(4):
            js = slice(j * 128, (j + 1) * 128)
            A("dve", lambda e, js=js: e.tensor_tensor_scan(out=acr[:, js], data0=onesrow[:, js], data1=dAr[:, js], initial=0.0,
                                                          op0=ALU.mult, op1=ALU.add), reads=[u_dAr, u_onesrow], writes=[u_acr])
        A("dve", lambda e: e.tensor_copy(out=alast[:], in_=acr[:, 127::128]), reads=[u_acr], writes=[u_alast])
        pc, pcu = C.next_bank()
        for j in range(4):
            js = slice(j * 128, (j + 1) * 128)
            A("pe", lambda e, j=j, js=js, pc=pc: e.matmul(pc[:, 2 * j:2 * j + 2], lhsT=acr[:, js], rhs=ident[0:2, 0:2],
                                                         start=True, stop=True), reads=[u_acr, u_ident], writes=[pcu])
            A("pe", lambda e, j=j, js=js, pc=pc: e.matmul(pc[:, 8 + 2 * j:8 + 2 * j + 2], lhsT=dtr[:, js], rhs=ident[0:2, 0:2],
                                                         start=True, stop=True), reads=[u_dtr, u_ident], writes=[pcu])
        for h in range(2):
            A("pe", lambda e, h=h, pc=pc: e.matmul(pc[:, 16 + 4 * h:16 + 4 * h + 4], lhsT=sel[h][0][:], rhs=alast[:],
                                                  start=True, stop=True), reads=[sel[h][1], u_alast], writes=[pcu])
        A("dve", lambda e, pc=pc: e.tensor_copy(out=colA[:].rearrange("p c h -> p (c h)"), in_=pc[:, 0:8]), reads=[pcu], writes=[u_colA])
        A("dve", lambda e, pc=pc: e.tensor_copy(out=colD[:].rearrange("p c h -> p (c h)"), in_=pc[:, 8:16]), reads=[pcu], writes=[u_colD])
        A("dve", lambda e, pc=pc: e.tensor_copy(out=colL[:].rearrange("p h c -> p (h c)"), in_=pc[:, 16:24]), reads=[pcu], writes=[u_colL])
        A("dve", lambda e: e.tensor_tensor(out=colW[:], in0=colL[:].rearrange("p h c -> p c h"), in1=colA[:], op=ALU.subtract),
          reads=[u_colL, u_colA], writes=[u_colW])
        A("act", lambda e: e.activation(out=colW[:], in_=colW[:], func=AF.Exp), reads=[u_colW], writes=[u_colW])
        A("dve", lambda e: e.tensor_tensor(out=colW[:], in0=colW[:], in1=colD[:], op=ALU.mult), reads=[u_colW, u_colD], writes=[u_colW])
        A("act", lambda e: e.activation(out=colE[:], in_=colL[:], func=AF.Exp), reads=[u_colL], writes=[u_colE])
        for src, dst, du in ((cout[0], xtok, u_xtok), (cout[1], Btok, u_Btok)):
            tb, tbu = C.next_bank()
            for j in range(4):
                A("pe", lambda e, j=j, tb=tb, src=src: e.transpose(out=tb[:, j * 128:(j + 1) * 128], in_=src[0][:, j * 128:(j + 1) * 128],
                                                                  identity=ident[:]), reads=[src[1], u_ident], writes=[tbu])
            A("act", lambda e, tb=tb, dst=dst: e.copy(out=dst[:], in_=tb[:].rearrange("p (j c) -> p j c", j=4)), reads=[tbu], writes=[du])
        yo, you = ygo[sb % 2]
        for j in range(4):
            js = slice(j * 128, (j + 1) * 128)
            cbk, cbku = C.next_bank()
            A("pe", lambda e, js=js, cbk=cbk: e.matmul(cbk[:, 0:128], lhsT=BTb[:, js], rhs=CTb[:, js], start=True, stop=True),
              reads=[u_BTb, u_CTb], writes=[cbku])
            ark, arku = C.next_bank()
            for h in range(2):
                A("pe", lambda e, h=h, js=js, ark=ark: e.matmul(ark[:, h * 128:(h + 1) * 128], lhsT=sel[h][0][:], rhs=acr[:, js],
                                                               start=True, stop=True), reads=[sel[h][1], u_acr], writes=[arku])
            for h in range(2):
                A("dve", lambda e, h=h, j=j: e.tensor_scalar(out=xw[:, h * 64:(h + 1) * 64], in0=xtok[:, j, h * 64:(h + 1) * 64],
                                                             scalar1=colW[:, j, h:h + 1], scalar2=None, op0=ALU.mult),
                  reads=[u_xtok, u_colW], writes=[u_xw])
            stk, stku = C.next_bank()
            A("pe", lambda e, j=j, stk=stk: e.matmul(stk[:, 0:128], lhsT=Btok[:, j, :], rhs=xw[:], start=True, stop=True),
              reads=[u_Btok, u_xw], writes=[stku])
            yk, yku = C.next_bank()
            for h in range(2):
                lt, ltu = LT[h]; lm, lmu = Lm[h]; mt, mtu = MT[h]; ee, eeu = Ee[h]; ce, ceu = CE[h]
                hs = slice(h * 64, (h + 1) * 64)
                A("dve", lambda e, h=h, j=j: e.tensor_scalar(out=colA[:, j, h:h + 1], in0=colA[:, j, h:h + 1], scalar1=-1.0, scalar2=None,
                                                             op0=ALU.mult), reads=[u_colA], writes=[u_colA])
                A("act", lambda e, h=h, j=j, ark=ark, lt=lt: e.activation(out=lt[:], in_=ark[:, h * 128:(h + 1) * 128], func=AF.Identity,
                                                                          bias=colA[:, j, h:h + 1], scale=1.0),
                  reads=[arku, u_colA], writes=[ltu])
                A("dve", lambda e, lt=lt: e.tensor_scalar(out=lt[:], in0=lt[:], scalar1=0.0, scalar2=None, op0=ALU.min),
                  reads=[ltu], writes=[ltu])
                A("act", lambda e, lt=lt: e.activation(out=lt[:], in_=lt[:], func=AF.Exp), reads=[ltu], writes=[ltu])
                A("dve", lambda e, h=h, j=j, lt=lt, lm=lm: e.scalar_tensor_tensor(out=lm[:], in0=lt[:], scalar=colD[:, j, h:h + 1], in1=maskF[:],
                                                                                  op0=ALU.mult, op1=ALU.mult),
                  reads=[ltu, u_colD, u_maskF], writes=[lmu])
                A("dve", lambda e, cbk=cbk, lm=lm, mt=mt: e.tensor_tensor(out=mt[:], in0=cbk[:, 0:128], in1=lm[:], op=ALU.mult),
                  reads=[cbku, lmu], writes=[mtu])
                A("act", lambda e, h=h, ark=ark, ee=ee: e.activation(out=ee[:], in_=ark[:, h * 128:(h + 1) * 128], func=AF.Exp),
                  reads=[arku], writes=[eeu])
                A("pool", lambda e, js=js, ee=ee, ce=ce: e.tensor_tensor(out=ce[:], in0=cout[2][0][:, js], in1=ee[:], op=ALU.mult),
                  reads=[cout[2][1], eeu], writes=[ceu])
                A("pe", lambda e, j=j, hs=hs, yk=yk, mt=mt: e.matmul(yk[hs, 0:128], lhsT=xtok[:, j, hs], rhs=mt[:], start=True, stop=False),
                  reads=[mtu, u_xtok], writes=[yku])
                A("pe", lambda e, hs=hs, yk=yk, ce=ce: e.matmul(yk[hs, 0:128], lhsT=HTb[:, hs], rhs=ce[:], start=False, stop=True),
                  reads=[ceu, u_HTb], writes=[yku])
            A("dve", lambda e, js=js, yk=yk: e.scalar_tensor_tensor(out=ysb[:], in0=cout[0][0][:, js], scalar=dskc[:, 0:1],
                                                                   in1=yk[:, 0:128], op0=ALU.mult, op1=ALU.add),
              reads=[yku, cout[0][1], u_dskc], writes=[u_ysb])
            A("pool", lambda e, js=js, yo=yo: e.tensor_tensor(out=yo[:, js], in0=ysb[:], in1=zsT[:, js], op=ALU.mult),
              reads=[u_ysb, u_zsT], writes=[you])
            for h in range(2):
                hs = slice(h * 64, (h + 1) * 64)
                A("dve", lambda e, h=h, j=j, hs=hs, stk=stk: e.scalar_tensor_tensor(out=HT[:, hs], in0=HT[:, hs], scalar=colE[:, h, j:j + 1],
                                                                                   in1=stk[:, hs], op0=ALU.mult, op1=ALU.add),
                  reads=[stku, u_HT, u_colE], writes=[u_HT])
            A("pool", lambda e: e.tensor_copy(out=HTb[:], in_=HT[:]), reads=[u_HT], writes=[u_HTb])
        A("sp", lambda e, yo=yo, tsl=tsl: e.dma_start(out=T["ygT"][:, tsl], in_=yo[:]), reads=[you], group=DmaGroup(ygo_l[sb % 2]))

    fk, fku = C.next_bank()
    A("pe", lambda e: e.matmul(fk[:, 0:NQB + 4], lhsT=ones_f[0:1, :], rhs=Frow[:], start=True, stop=True),
      reads=[u_onesf, u_Frow], writes=[fku])
    A("dve", lambda e: e.tensor_copy(out=Frep[:], in_=fk[:, 0:NQB + 4]), reads=[fku], writes=[u_Frep])
    offs = []
    oo = 0
    for i in range(NQB):
        offs.append(oo)
        A("dve", lambda e, i=i, oo=oo: e.tensor_scalar(out=Btab[:, oo:oo + i + 1], in0=cT[:, 0:i + 1], scalar1=-1.0, scalar2=Frep[:, i:i + 1],
                                                      op0=ALU.mult, op1=ALU.add), reads=[u_cT, u_Frep], writes=[u_Btab])
        oo += i + 1
    res = [C.reserve_bank() for _ in range(4)]
    pairs = [(i, j) for i in range(NQB) for j in range(i + 1)]
    sbanks = {}
    def issue_s(k):
        i, j = pairs[k]
        bk, bu = C.next_bank()
        sbanks[k] = (bk, bu)
        A("pe", lambda e, i=i, j=j, bk=bk: e.matmul(bk[:, 0:128], lhsT=kT[:, j * 128:(j + 1) * 128], rhs=qT[:, i * 128:(i + 1) * 128],
                                                    start=True, stop=True), reads=[u_kT, u_qT], writes=[bu])
    DEPTH_S = 2
    for k in range(min(DEPTH_S, len(pairs))):
        issue_s(k)
    for k, (i, j) in enumerate(pairs):
        if k + DEPTH_S < len(pairs):
            issue_s(k + DEPTH_S)
        bk, bu = sbanks.pop(k)
        pt, ptu = PT[k % NPT]
        (ab, abu) = res[(i % 2) * 2][1]
        (db, dbu) = res[(i % 2) * 2 + 1][1]
        col = offs[i] + j
        A("act", lambda e, bk=bk, pt=pt, col=col: e.activation(out=pt[:], in_=bk[:, 0:128], func=AF.Exp, bias=Btab[:, col:col + 1],
                                                              scale=ATT_SCALE), reads=[bu, u_Btab], writes=[ptu])
        if j == i:
            A("pool", lambda e, pt=pt: e.tensor_tensor(out=pt[:], in0=pt[:], in1=maskT[:], op=ALU.mult), reads=[ptu, u_mask], writes=[ptu])
        A("pe", lambda e, j=j, i=i, ab=ab, pt=pt: e.matmul(ab[:, 0:128], lhsT=Vt[:, j, :], rhs=pt[:], start=(j == 0), stop=(j == i)),
          reads=[u_Vt, ptu], writes=[abu])
        A("pe", lambda e, j=j, i=i, db=db, pt=pt: e.matmul(db[:, 0:128], lhsT=ones_bf[:], rhs=pt[:], start=(j == 0), stop=(j == i)),
          reads=[u_ones, ptu], writes=[dbu])
        if j == i:
            at, atu = ato[(i // 4) % 2]
            A("dve", lambda e, db=db: e.reciprocal(out=rec[:], in_=db[:, 0:128]), reads=[dbu], writes=[u_rec])
            A("dve", lambda e, ab=ab, at=at, i=i: e.tensor_tensor(out=at[:, (i % 4) * 128:(i % 4 + 1) * 128], in0=ab[:, 0:128], in1=rec[:],
                                                                  op=ALU.mult), reads=[abu, u_rec], writes=[atu])
            if i % 4 == 3:
                s4 = slice((i // 4) * 512, (i // 4 + 1) * 512)
                A("sp", lambda e, at=at, s4=s4: e.dma_start(out=T["attT"][:, s4], in_=at[:]), reads=[atu],
                  group=DmaGroup(ato_l[(i // 4) % 2]))
    for r in res:
        C.release_bank(r[0])


def stage_a_program():
    C = Ctx()
    T = {}
    T["hT"] = C.dram_in("hT", [D, SEQ])
    T["wA"] = C.dram_in("wA", [128, 16, 896])
    T["wA2"] = C.dram_in("wA2", [128, 16, 4])
    T["ident"] = C.dram_in("ident", [128, 128])
    T["maskT"] = C.dram_in("maskT", [128, 128])
    T["convw"] = C.dram_in("convw", [128, 3, 4])
    T["convb"] = C.dram_in("convb", [128, 3])
    T["dsk"] = C.dram_in("dsk", [128, 1])
    T["bf"] = C.dram_in("bf", [1, 1])
    T["dtb"] = C.dram_in("dtb", [2, 1])
    T["alog"] = C.dram_in("alog", [2, 1])
    T["sel"] = C.dram_in("sel", [2, 2, 128])
    T["attT"] = C.dram_out("attT", [128, SEQ])
    T["ygT"] = C.dram_out("ygT", [128, SEQ])
    build_stage_a(C, T)
    return C.finish()


def stage_a_inputs(inp, i, c):
    f = np.ascontiguousarray
    w_in = inp["w_in"][i]
    g = c // 2
    cols = []
    for base in (0, 1024, 2048):
        cols.append(np.arange(base + 128 * c, base + 128 * c + 128))
    cols.append(np.arange(3080 + 128 * c, 3080 + 128 * c + 128))
    cols.append(np.arange(4104 + 128 * c, 4104 + 128 * c + 128))
    cols.append(np.arange(4104 + 1024 + 128 * g, 4104 + 1024 + 128 * g + 128))
    cols.append(np.arange(4104 + 1536 + 128 * g, 4104 + 1536 + 128 * g + 128))
    cols = np.concatenate(cols)
    m = {}
    m["wA"] = f(w_in[:, cols].reshape(16, 128, 896).transpose(1, 0, 2))
    c2 = np.array([3072 + c, 6152 + 2 * c, 6152 + 2 * c + 1, 6152 + 2 * c + 1])
    m["wA2"] = f(w_in[:, c2].reshape(16, 128, 4).transpose(1, 0, 2))
    m["ident"] = np.eye(128, dtype=np.float32)
    m["maskT"] = np.triu(np.ones((128, 128), dtype=np.float32))
    ch = [np.arange(128 * c, 128 * c + 128), np.arange(1024 + 128 * g, 1024 + 128 * g + 128),
          np.arange(1536 + 128 * g, 1536 + 128 * g + 128)]
    m["convw"] = f(np.stack([inp["conv_w"][i][:, cc].T for cc in ch], axis=1))
    m["convb"] = f(np.stack([inp["conv_b"][i][cc] for cc in ch], axis=1))
    m["dsk"] = f(np.repeat(inp["d_skip"][i][2 * c:2 * c + 2], 64).reshape(128, 1))
    m["bf"] = f(inp["b_forget"][i][c:c + 1].reshape(1, 1))
    m["dtb"] = f(inp["dt_bias"][i][2 * c:2 * c + 2].reshape(2, 1))
    m["alog"] = f(inp["a_log"][i][2 * c:2 * c + 2].reshape(2, 1))
    sel = np.zeros((2, 2, 128), dtype=np.float32)
    sel[0, 0, :] = 1.0
    sel[1, 1, :] = 1.0
    m["sel"] = sel
    return m


def run_stage_a(nc, inp, i, h):
    hT = np.ascontiguousarray(h.T)
    in_maps = []
    for c in range(NCORES):
        m = stage_a_inputs(inp, i, c)
        m["hT"] = hT
        in_maps.append(m)
    res = run_bass_kernel_spmd(nc, in_maps, core_ids=list(range(NCORES)))
    attT = np.stack([r["attT"] for r in res.results])
    ygT = np.stack([r["ygT"] for r in res.results])
    return attT, ygT


def kernel(**inputs):
    inp = {k: np.asarray(v) for k, v in inputs.items()}
    h = np.ascontiguousarray(inp["x"][0], dtype=np.float32)
    ncA = stage_a_program()
    ncB = stage_b_program()
    for i in range(DEPTH):
        attT, ygT = run_stage_a(ncA, inp, i, h)
        mixedT = np.concatenate([attT.reshape(1024, SEQ), ygT.reshape(1024, SEQ)], axis=0)
        h = run_stage_b(ncB, inp, i, h, mixedT)
    return np.ascontiguousarray(h[None], dtype=np.float32)
```
